# Optimizing a Trainium2 kernel written in Bass

```python
import math
import numpy as np
import jax
import jax.numpy as jnp
from jax import lax

D_MODEL = 1024
BATCH = 4
SEQ = 4096
DEPTH = 4

HEAD_DIM = 64
QBLK = 128
SPARSE_QCHUNK = 32
NEG_INF = -1e30
EPS = 1e-6

NSA_HEADS = 4
CMP_BLOCK = 32
CMP_STRIDE = 16
CMP_HIDDEN = 256
SEL_BLOCK = 64
SEL_TOPK = 8
NSA_WINDOW = 512
FORCE_BONUS = 1e4
SWA_HEADS = 4
SWA_KV_HEADS = 2
SWA_WINDOW = 128
MOBA_HEADS = 4
MOBA_BLOCK = 256
MOBA_TOPK = 3
MLA_HEADS = 4
MLA_Q_RANK = 384
MLA_KV_RANK = 128
MLA_NOPE = 64
MLA_ROPE = 32
MLA_V = 64
ROPE_THETA = 10000.0
N_BRANCH = 4
BRANCH_WIDTH = 256
N_GROUPS = 4
EXPERTS_PER_GROUP = 8
N_EXPERTS = N_GROUPS * EXPERTS_PER_GROUP
TOPK_IN_GROUP = 2
D_EXPERT = 256
MOE_BLOCK = 128

QK_NSA_Q = 0
QK_NSA_KC = 1
QK_NSA_KS = 2
QK_NSA_KW = 3
QK_SWA_Q = 4
QK_SWA_K = 5
QK_MOBA_Q = 6
QK_MOBA_K = 7
QK_MLA_Q = 8
QK_MLA_K = 9
N_QK = 10

IN_SIZES = (
    NSA_HEADS * HEAD_DIM,
    HEAD_DIM, HEAD_DIM,
    HEAD_DIM, HEAD_DIM,
    HEAD_DIM, HEAD_DIM,
    3 * NSA_HEADS,
    SWA_HEADS * HEAD_DIM,
    SWA_KV_HEADS * HEAD_DIM,
    SWA_KV_HEADS * HEAD_DIM,
    MOBA_HEADS * HEAD_DIM,
    MOBA_HEADS * HEAD_DIM,
    MOBA_HEADS * HEAD_DIM,
    MLA_Q_RANK,
    MLA_KV_RANK,
    MLA_ROPE,
    N_BRANCH * D_MODEL,
)
D_IN = sum(IN_SIZES)

kernel_name = 'hybrid_nsa_swa_moba_mla_hmoe_trunk'


def rms_norm(x, g):
    xf = x.astype(jnp.float32)
    y = xf * lax.rsqrt(jnp.mean(xf * xf, axis=-1, keepdims=True) + EPS)
    return (y * g.astype(jnp.float32)).astype(x.dtype)


def alibi_slopes():
    n = NSA_HEADS + SWA_HEADS + MOBA_HEADS
    def pow2(m):
        start = 2.0 ** (-8.0 / m)
        return [start ** (i + 1) for i in range(m)]
    c = 2 ** int(math.floor(math.log2(n)))
    s = pow2(c) + (pow2(2 * c)[0::2][: n - c] if c < n else [])
    s = -np.sort(-np.asarray(s, np.float32))
    return s.reshape(NSA_HEADS, 3).T


def to_chunks(a, n_chunks):
    return jnp.moveaxis(a.reshape(a.shape[0], n_chunks, -1, *a.shape[2:]), 1, 0)


def from_chunks(a):
    return jnp.moveaxis(a, 0, 1).reshape(a.shape[1], -1, *a.shape[3:])


def apply_rope(x, pos):
    half = x.shape[-1] // 2
    inv = ROPE_THETA ** (-jnp.arange(half, dtype=jnp.float32) / half)
    ang = pos.astype(jnp.float32)[:, None] * inv[None, :]
    cos = jnp.cos(ang)[:, None, :]
    sin = jnp.sin(ang)[:, None, :]
    xf = x.astype(jnp.float32)
    x1, x2 = xf[..., :half], xf[..., half:]
    return jnp.concatenate([x1 * cos - x2 * sin, x1 * sin + x2 * cos], axis=-1).astype(x.dtype)


def banded_attention(q, k, v, window, slopes, sinks):
    B, S, H, dh = q.shape
    G = k.shape[2]
    R = H // G
    f32 = jnp.float32
    nq = S // QBLK
    span = window + QBLK
    pad = ((0, 0), (window, 0), (0, 0), (0, 0))
    idx = (jnp.arange(nq) * QBLK)[:, None] + jnp.arange(span)[None, :]
    kb = jnp.pad(k, pad)[:, idx]
    vb = jnp.pad(v, pad)[:, idx]
    qb = q.reshape(B, nq, QBLK, G, R, dh)
    s = jnp.einsum('bnqgrd,bnkgd->bngrqk', qb, kb, preferred_element_type=f32) * (dh ** -0.5)
    tq = (jnp.arange(nq) * QBLK)[:, None] + jnp.arange(QBLK)[None, :]
    kpos = idx - window
    dist = tq[:, :, None] - kpos[:, None, :]
    valid = (dist >= 0) & (dist < window) & (kpos[:, None, :] >= 0)
    sl = slopes.reshape(G, R)[None, None, :, :, None, None]
    s = s - sl * dist.astype(f32)[None, :, None, None]
    s = jnp.where(valid[None, :, None, None], s, NEG_INF)
    if sinks is None:
        p = jax.nn.softmax(s, axis=-1)
    else:
        sk = sinks.astype(f32).reshape(G, R)[None, None, :, :, None, None]
        m = jnp.maximum(jnp.max(s, axis=-1, keepdims=True), sk)
        e = jnp.exp(s - m)
        p = e / (jnp.sum(e, axis=-1, keepdims=True) + jnp.exp(sk - m))
    out = jnp.einsum('bngrqk,bnkgd->bnqgrd', p.astype(v.dtype), vb)
    return out.reshape(B, S, H, dh)


def compress_blocks(kv, pe, w1, w2):
    B, S, dk = kv.shape
    n_cmp = (S - CMP_BLOCK) // CMP_STRIDE + 1
    idx = (jnp.arange(n_cmp) * CMP_STRIDE)[:, None] + jnp.arange(CMP_BLOCK)[None, :]
    blk = (kv[:, idx] + pe).reshape(B, n_cmp, CMP_BLOCK * dk)
    return jax.nn.silu(blk @ w1) @ w2


def selected_block_attention(q, k, v, sel_idx, sel_ok, slopes):
    B, S, H, dk = q.shape
    n = sel_idx.shape[-1]
    f32 = jnp.float32
    nch = S // SPARSE_QCHUNK
    kblk = k.reshape(B, S // SEL_BLOCK, SEL_BLOCK, dk)
    vblk = v.reshape(B, S // SEL_BLOCK, SEL_BLOCK, dk)
    bi = jnp.arange(B)[:, None, None]
    off = jnp.arange(SEL_BLOCK)

    def chunk(args):
        qi, ii, oi, ci = args
        kg = kblk[bi, ii]
        vg = vblk[bi, ii]
        s = jnp.einsum('bchd,bcnld->bchnl', qi, kg, preferred_element_type=f32) * (dk ** -0.5)
        t = ci * SPARSE_QCHUNK + jnp.arange(SPARSE_QCHUNK)
        dist = t[None, :, None, None] - (ii[..., None] * SEL_BLOCK + off)
        valid = oi[..., None] & (dist >= 0)
        s = s - slopes[None, None, :, None, None] * dist[:, :, None].astype(f32)
        s = jnp.where(valid[:, :, None], s, NEG_INF).reshape(B, SPARSE_QCHUNK, H, n * SEL_BLOCK)
        p = jax.nn.softmax(s, axis=-1).astype(v.dtype)
        return jnp.einsum('bchk,bckd->bchd', p, vg.reshape(B, SPARSE_QCHUNK, n * SEL_BLOCK, dk))

    out = lax.map(chunk, (to_chunks(q, nch), to_chunks(sel_idx, nch), to_chunks(sel_ok, nch), jnp.arange(nch)))
    return from_chunks(out)


def nsa_attention(q, kc_raw, vc_raw, ks, vs, kw, vw, gate_logit, g_kc, pe, w1, w2, slopes):
    B, S, H, dk = q.shape
    f32 = jnp.float32
    t = jnp.arange(S)
    kc = rms_norm(compress_blocks(kc_raw, pe[0], w1[0], w2[0]), g_kc)
    vc = compress_blocks(vc_raw, pe[1], w1[1], w2[1])
    n_cmp = kc.shape[1]
    c_end = jnp.arange(n_cmp) * CMP_STRIDE + CMP_BLOCK - 1
    dist = t[:, None] - c_end[None, :]
    vis = dist >= 0
    s = jnp.einsum('bshd,bnd->bhsn', q, kc, preferred_element_type=f32) * (dk ** -0.5)
    s = s - slopes[None, :, None, None] * dist.astype(f32)
    s = jnp.where(vis, s, NEG_INF)
    e = jnp.exp(s - jnp.max(s, axis=-1, keepdims=True)) * vis
    p_cmp = e / jnp.maximum(jnp.sum(e, axis=-1, keepdims=True), 1e-30)
    o_cmp = jnp.einsum('bhsn,bnd->bshd', p_cmp.astype(vc.dtype), vc)
    n_sel = S // SEL_BLOCK
    starts = np.arange(n_cmp) * CMP_STRIDE
    jb = np.arange(n_sel) * SEL_BLOCK
    cover = ((starts[:, None] < jb[None, :] + SEL_BLOCK) & (starts[:, None] + CMP_BLOCK > jb[None, :])).astype(np.float32)
    p_slc = jnp.einsum('bhsn,nj->bsj', p_cmp, jnp.asarray(cover))
    cur = (t // SEL_BLOCK)[:, None]
    j = jnp.arange(n_sel)[None, :]
    forced = (j == 0) | (j == cur) | (j == cur - 1)
    score = jnp.where(j <= cur, p_slc + FORCE_BONUS * forced.astype(f32), NEG_INF)
    top_val, top_idx = lax.top_k(score, min(SEL_TOPK, n_sel))
    o_slc = selected_block_attention(q, ks, vs, top_idx, top_val > 0.5 * NEG_INF, slopes)
    o_win = banded_attention(q, kw[:, :, None], vw[:, :, None], NSA_WINDOW, slopes, None)
    g = jax.nn.sigmoid(gate_logit.astype(f32)).reshape(B, S, H, 3).astype(q.dtype)
    return g[..., 0:1] * o_cmp + g[..., 1:2] * o_slc + g[..., 2:3] * o_win


def moba_attention(q, k, v, slopes):
    B, S, H, dh = q.shape
    f32 = jnp.float32
    L = MOBA_BLOCK
    nb = -(-S // L)
    pad = ((0, 0), (0, nb * L - S), (0, 0), (0, 0))
    kp = jnp.pad(k, pad)
    vp = jnp.pad(v, pad)
    kblk = kp.reshape(B, nb, L, H, dh)
    kmean = jnp.mean(kblk.astype(f32), axis=2)
    t = jnp.arange(S)
    gs = jnp.einsum('bshd,bnhd->bshn', q.astype(f32), kmean)
    past = jnp.arange(nb)[None, :] < (t // L)[:, None]
    gs = jnp.where(past[None, :, None, :], gs, NEG_INF)
    kk = min(MOBA_TOPK, nb)
    top_val, top_idx = lax.top_k(gs, kk)
    top_ok = top_val > 0.5 * NEG_INF
    kbh = jnp.transpose(kblk, (0, 3, 1, 2, 4))
    vbh = jnp.transpose(vp.reshape(B, nb, L, H, dh), (0, 3, 1, 2, 4))
    bi = jnp.arange(B)[:, None, None, None]
    hi = jnp.arange(H)[None, None, :, None]
    off = jnp.arange(L)
    scale = dh ** -0.5
    C = SPARSE_QCHUNK
    nch = S // C

    def chunk(args):
        qi, ii, oi, ci = args
        tc = ci * C + jnp.arange(C)
        kg = kbh[bi, hi, ii]
        vg = vbh[bi, hi, ii]
        s_past = jnp.einsum('bchd,bchild->bchil', qi, kg, preferred_element_type=f32) * scale
        d_past = tc[None, :, None, None, None] - (ii[..., None] * L + off)
        s_past = s_past - slopes[None, None, :, None, None] * d_past.astype(f32)
        s_past = jnp.where(oi[..., None], s_past, NEG_INF).reshape(B, C, H, kk * L)
        start = (ci * C // L) * L
        ko = lax.dynamic_slice_in_dim(kp, start, L, axis=1)
        vo = lax.dynamic_slice_in_dim(vp, start, L, axis=1)
        s_own = jnp.einsum('bchd,blhd->bchl', qi, ko, preferred_element_type=f32) * scale
        d_own = tc[:, None] - (start + off)[None, :]
        s_own = s_own - slopes[None, None, :, None] * d_own.astype(f32)[None, :, None, :]
        s_own = jnp.where((d_own >= 0)[None, :, None, :], s_own, NEG_INF)
        p = jax.nn.softmax(jnp.concatenate([s_past, s_own], axis=-1), axis=-1).astype(v.dtype)
        o_past = jnp.einsum('bchk,bchkd->bchd', p[..., : kk * L], vg.reshape(B, C, H, kk * L, dh))
        o_own = jnp.einsum('bchl,blhd->bchd', p[..., kk * L:], vo)
        return o_past + o_own

    out = lax.map(chunk, (to_chunks(q, nch), to_chunks(top_idx, nch), to_chunks(top_ok, nch), jnp.arange(nch)))
    return from_chunks(out)


def causal_attention(q, k, v):
    B, S, H, dq = q.shape
    nq = S // QBLK
    kpos = jnp.arange(S)

    def block(args):
        qi, bidx = args
        s = jnp.einsum('bqhd,bkhd->bhqk', qi, k, preferred_element_type=jnp.float32) * (dq ** -0.5)
        tq = bidx * QBLK + jnp.arange(QBLK)
        s = jnp.where((kpos[None, :] <= tq[:, None])[None, None], s, NEG_INF)
        p = jax.nn.softmax(s, axis=-1).astype(v.dtype)
        return jnp.einsum('bhqk,bkhd->bqhd', p, v)

    out = lax.map(block, (to_chunks(q, nq), jnp.arange(nq)))
    return from_chunks(out)


def mla_attention(cq, ckv, k_rope, g_cq, g_ckv, w_uq, w_ukv, g_qn, g_kn, g_rope, pos):
    B, S, _ = cq.shape
    q = (rms_norm(cq, g_cq) @ w_uq).reshape(B, S, MLA_HEADS, MLA_NOPE + MLA_ROPE)
    kv = (rms_norm(ckv, g_ckv) @ w_ukv).reshape(B, S, MLA_HEADS, MLA_NOPE + MLA_V)
    q_nope = rms_norm(q[..., :MLA_NOPE], g_qn)
    q_rot = apply_rope(rms_norm(q[..., MLA_NOPE:], g_rope[0]), pos)
    k_nope = rms_norm(kv[..., :MLA_NOPE], g_kn)
    v = kv[..., MLA_NOPE:]
    k_rot = apply_rope(rms_norm(k_rope, g_rope[1])[:, :, None, :], pos)
    qf = jnp.concatenate([q_nope, q_rot], axis=-1)
    kf = jnp.concatenate([k_nope, jnp.broadcast_to(k_rot, (B, S, MLA_HEADS, MLA_ROPE))], axis=-1)
    return causal_attention(qf, kf, v)


def routed_experts(xf, eid, wts, w13, w2):
    N, D = xf.shape
    K = eid.shape[1]
    E = w13.shape[0]
    flat_e = eid.reshape(-1)
    order = jnp.argsort(flat_e)
    se = flat_e[order]
    counts = jnp.bincount(flat_e, length=E)
    padded = (counts + MOE_BLOCK - 1) // MOE_BLOCK * MOE_BLOCK
    pend = jnp.cumsum(padded)
    pstart = pend - padded
    start = jnp.cumsum(counts) - counts
    dest = pstart[se] + jnp.arange(N * K) - start[se]
    n_blocks = -(-(N * K) // MOE_BLOCK) + E
    R = n_blocks * MOE_BLOCK
    row_tok = jnp.full((R,), N, jnp.int32).at[dest].set((order // K).astype(jnp.int32))
    row_w = jnp.zeros((R,), xf.dtype).at[dest].set(wts.reshape(-1)[order])
    blk_e = jnp.minimum(jnp.searchsorted(pend, jnp.arange(n_blocks) * MOE_BLOCK, side='right'), E - 1)
    xpad = jnp.concatenate([xf, jnp.zeros((1, D), xf.dtype)], axis=0)
    xb = xpad[row_tok].reshape(n_blocks, MOE_BLOCK, D)

    def block(args):
        xi, e = args
        a, b = jnp.split(xi @ w13[e], 2, axis=-1)
        return (jax.nn.silu(a) * b) @ w2[e]

    yb = lax.map(block, (xb, blk_e)).reshape(R, D)
    y = jnp.zeros((N + 1, D), xf.dtype).at[row_tok].add(yb * row_w[:, None])
    return y[:N]


def hier_moe(h, w_coarse, b_coarse, w_fine, b_fine, w13, w2):
    B, S, D = h.shape
    N = B * S
    f32 = jnp.float32
    xf = h.reshape(N, D)
    lc = (xf @ w_coarse).astype(f32) + b_coarse.astype(f32)
    grp = jnp.argmax(lc, axis=-1)
    p_grp = jnp.take_along_axis(jax.nn.softmax(lc, axis=-1), grp[:, None], axis=-1)
    lf = ((xf @ w_fine).astype(f32) + b_fine.astype(f32)).reshape(N, N_GROUPS, EXPERTS_PER_GROUP)
    lf = jnp.take_along_axis(lf, grp[:, None, None], axis=1)[:, 0]
    top_p, top_e = lax.top_k(jax.nn.softmax(lf, axis=-1), TOPK_IN_GROUP)
    wts = p_grp * top_p / jnp.sum(top_p, axis=-1, keepdims=True)
    eid = (grp[:, None] * EXPERTS_PER_GROUP + top_e).astype(jnp.int32)
    return routed_experts(xf, eid, wts.astype(h.dtype), w13, w2).reshape(B, S, D)


def setup_inputs(seed: int = 0) -> dict:
    key = jax.random.key(seed)
    ks = jax.random.split(key, 24)
    f32 = jnp.float32
    L = DEPTH
    D = D_MODEL

    def nrm(k, shape, fan_in, s=1.0):
        return (s * fan_in ** -0.5) * jax.random.normal(k, shape, f32)

    def gain(k, shape):
        return 1.0 + 0.05 * jax.random.normal(k, shape, f32)

    return {
        'x': jax.random.normal(ks[0], (BATCH, SEQ, D), f32),
        'c': jax.random.normal(ks[1], (BATCH, D), f32),
        'w_ada': nrm(ks[2], (L, D, 6 * D), D, 0.5),
        'b_ada': 0.02 * jax.random.normal(ks[3], (L, 6 * D), f32),
        'norm_gain': gain(ks[4], (L, 2, D)),
        'w_in': nrm(ks[5], (L, D, D_IN), D),
        'qk_gain': gain(ks[6], (L, N_QK, HEAD_DIM)),
        'cmp_pe': 0.02 * jax.random.normal(ks[7], (L, 2, CMP_BLOCK, HEAD_DIM), f32),
        'cmp_w1': nrm(ks[8], (L, 2, CMP_BLOCK * HEAD_DIM, CMP_HIDDEN), CMP_BLOCK * HEAD_DIM),
        'cmp_w2': nrm(ks[9], (L, 2, CMP_HIDDEN, HEAD_DIM), CMP_HIDDEN),
        'swa_sinks': jax.random.normal(ks[10], (L, SWA_HEADS), f32),
        'lat_gain_q': gain(ks[11], (L, MLA_Q_RANK)),
        'lat_gain_kv': gain(ks[12], (L, MLA_KV_RANK)),
        'rope_gain': gain(ks[13], (L, 2, MLA_ROPE)),
        'w_uq': nrm(ks[14], (L, MLA_Q_RANK, MLA_HEADS * (MLA_NOPE + MLA_ROPE)), MLA_Q_RANK),
        'w_ukv': nrm(ks[15], (L, MLA_KV_RANK, MLA_HEADS * (MLA_NOPE + MLA_V)), MLA_KV_RANK),
        'w_branch': nrm(ks[16], (L, N_BRANCH, BRANCH_WIDTH, D), BRANCH_WIDTH),
        'w_out': nrm(ks[17], (L, D, D), D),
        'w_coarse': nrm(ks[18], (L, D, N_GROUPS), D),
        'b_coarse': 0.01 * jax.random.normal(ks[19], (L, N_GROUPS), f32),
        'w_fine': nrm(ks[20], (L, D, N_EXPERTS), D),
        'b_fine': 0.01 * jax.random.normal(ks[21], (L, N_EXPERTS), f32),
        'w13': nrm(ks[22], (L, N_EXPERTS, D, 2 * D_EXPERT), D),
        'w2': nrm(ks[23], (L, N_EXPERTS, D_EXPERT, D), D_EXPERT),
    }


def reference(x, c, w_ada, b_ada, norm_gain, w_in, qk_gain, cmp_pe, cmp_w1, cmp_w2, swa_sinks,
              lat_gain_q, lat_gain_kv, rope_gain, w_uq, w_ukv, w_branch, w_out,
              w_coarse, b_coarse, w_fine, b_fine, w13, w2):
    B, S, D = x.shape
    slopes = jnp.asarray(alibi_slopes())
    pos = jnp.arange(S)
    split_at = np.cumsum(IN_SIZES)[:-1].tolist()
    for l in range(DEPTH):
        mod = jax.nn.silu(c) @ w_ada[l] + b_ada[l]
        sh1, sc1, g1, sh2, sc2, g2 = jnp.split(mod[:, None, :], 6, axis=-1)
        h = rms_norm(x, norm_gain[l, 0]) * (1.0 + sc1) + sh1
        (q_a, kc_a, vc_a, ks_a, vs_a, kw_a, vw_a, gl_a,
         q_b, k_b, v_b, q_c, k_c, v_c, cq_d, ckv_d, kr_d, gate_in) = jnp.split(h @ w_in[l], split_at, axis=-1)
        gq = qk_gain[l]
        o_a = nsa_attention(rms_norm(q_a.reshape(B, S, NSA_HEADS, HEAD_DIM), gq[QK_NSA_Q]),
                            kc_a, vc_a, rms_norm(ks_a, gq[QK_NSA_KS]), vs_a,
                            rms_norm(kw_a, gq[QK_NSA_KW]), vw_a, gl_a, gq[QK_NSA_KC],
                            cmp_pe[l], cmp_w1[l], cmp_w2[l], slopes[0])
        o_b = banded_attention(rms_norm(q_b.reshape(B, S, SWA_HEADS, HEAD_DIM), gq[QK_SWA_Q]),
                               rms_norm(k_b.reshape(B, S, SWA_KV_HEADS, HEAD_DIM), gq[QK_SWA_K]),
                               v_b.reshape(B, S, SWA_KV_HEADS, HEAD_DIM), SWA_WINDOW, slopes[1], swa_sinks[l])
        o_c = moba_attention(rms_norm(q_c.reshape(B, S, MOBA_HEADS, HEAD_DIM), gq[QK_MOBA_Q]),
                             rms_norm(k_c.reshape(B, S, MOBA_HEADS, HEAD_DIM), gq[QK_MOBA_K]),
                             v_c.reshape(B, S, MOBA_HEADS, HEAD_DIM), slopes[2])
        o_d = mla_attention(cq_d, ckv_d, kr_d, lat_gain_q[l], lat_gain_kv[l], w_uq[l], w_ukv[l],
                            gq[QK_MLA_Q], gq[QK_MLA_K], rope_gain[l], pos)
        branches = jnp.stack([o_a.reshape(B, S, BRANCH_WIDTH), o_b.reshape(B, S, BRANCH_WIDTH),
                              o_c.reshape(B, S, BRANCH_WIDTH), o_d.reshape(B, S, BRANCH_WIDTH)], axis=2)
        proj = jnp.einsum('bsnw,nwd->bsnd', branches, w_branch[l])
        gates = jax.nn.sigmoid(gate_in.reshape(B, S, N_BRANCH, D))
        mixed = jnp.sum(gates * proj, axis=2) @ w_out[l]
        x = x + g1 * mixed
        h2 = rms_norm(x, norm_gain[l, 1]) * (1.0 + sc2) + sh2
        x = x + g2 * hier_moe(h2, w_coarse[l], b_coarse[l], w_fine[l], b_fine[l], w13[l], w2[l])
    return x
```

```python
import math
import numpy as np
import ml_dtypes
import concourse.bass as bass
import concourse.mybir as mybir
from concourse.bass_utils import run_bass_kernel_spmd
from contextlib import ExitStack

F32 = mybir.dt.float32
BF16 = mybir.dt.bfloat16
F32R = mybir.dt.float32r
AF = mybir.ActivationFunctionType
ALU = mybir.AluOpType
AX = mybir.AxisListType

SEQ = 4096
D = 1024
NT = 32
DIN = 6572
NQKV = 2476
DEPTH = 4
NEG = -30000.0
EPS = 1e-6
NF = 2688
SB_FLOATS = 52000
PERSIST = 8192
NBLK = 96
SPARSE_MOE = True
I32 = mybir.dt.int32


class Op:
    __slots__ = ("eng", "fn", "dma", "deps", "signal", "sigval", "dsem", "dval", "idx")

    def __init__(self, eng, fn, dma):
        self.eng = eng
        self.fn = fn
        self.dma = dma
        self.deps = None
        self.signal = False
        self.sigval = 0
        self.dsem = None
        self.dval = 0


class Sched:
    ENGS = ("tensor", "vector", "scalar", "gpsimd", "sync")

    def __init__(self, nc, n_dma_sems=48):
        self.nc = nc
        self.ops = {e: [] for e in self.ENGS}
        self.last_w = {}
        self.readers = {}
        self.n_dma_sems = n_dma_sems
        self.dma_uses = [0] * n_dma_sems
        self.dma_last = [None] * n_dma_sems
        self.dma_rr = 0

    def add(self, eng, fn, reads=(), writes=(), dma=False):
        op = Op(eng, fn, dma)
        deps = {}

        def adddep(d):
            if d is op:
                return
            if d.dma:
                deps[("d", id(d))] = d
            else:
                k = ("e", d.eng)
                o = deps.get(k)
                if o is None or o.idx < d.idx:
                    deps[k] = d

        for r in reads:
            w = self.last_w.get(r)
            if w is not None:
                adddep(w)
        for w_ in writes:
            w = self.last_w.get(w_)
            if w is not None:
                adddep(w)
            rd = self.readers.get(w_)
            if rd:
                for d in rd.values():
                    adddep(d)
        if dma:
            i = self.dma_rr
            self.dma_rr = (self.dma_rr + 1) % self.n_dma_sems
            prev = self.dma_last[i]
            if prev is not None:
                adddep(prev)
            self.dma_uses[i] += 1
            op.dsem = i
            op.dval = 16 * self.dma_uses[i]
            self.dma_last[i] = op
        op.idx = len(self.ops[eng])
        op.deps = list(deps.values())
        for d in op.deps:
            d.signal = True
        for r in reads:
            rd = self.readers.get(r)
            if rd is None:
                rd = self.readers[r] = {}
            rd[("d", id(op)) if dma else ("e", eng)] = op
        for w_ in writes:
            self.last_w[w_] = op
            self.readers[w_] = {}
        self.ops[eng].append(op)
        return op

    def barrier(self):
        lasts = []
        for e in self.ENGS:
            for o in reversed(self.ops[e]):
                if o.fn is not None and not o.dma:
                    lasts.append(o)
                    break
        dmas = [d for d in self.dma_last if d is not None]
        for e in self.ENGS:
            op = Op(e, None, False)
            op.idx = len(self.ops[e])
            op.deps = list(lasts) + dmas
            for d in op.deps:
                d.signal = True
            self.ops[e].append(op)
        self.last_w = {}
        self.readers = {}

    def emit(self, final_waits=()):
        nc = self.nc
        with ExitStack() as es:
            esem = {e: es.enter_context(nc.semaphore("s_" + e)) for e in self.ENGS}
            dsem = [es.enter_context(nc.semaphore("d%d" % i)) for i in range(self.n_dma_sems)]
            for d in final_waits:
                d.signal = True
            for e in self.ENGS:
                c = 0
                for op in self.ops[e]:
                    if op.signal and not op.dma and op.fn is not None:
                        c += 1
                        op.sigval = c
            block = es.enter_context(nc.Block())

            def run(e, eng):
                known = {}
                for op in self.ops[e]:
                    for d in op.deps:
                        if d.dma:
                            key, sem, val = ("d", d.dsem), dsem[d.dsem], d.dval
                        else:
                            if d.eng == e and e == "tensor":
                                continue
                            key, sem, val = ("e", d.eng), esem[d.eng], d.sigval
                        if known.get(key, 0) >= val:
                            continue
                        eng.wait_ge(sem, val)
                        known[key] = val
                    if op.fn is None:
                        continue
                    inst = op.fn(eng)
                    if op.dma:
                        inst.then_inc(dsem[op.dsem], 16)
                    elif op.signal:
                        inst.then_inc(esem[e], 1)
                if e == "sync":
                    for d in final_waits:
                        if d.dma:
                            eng.wait_ge(dsem[d.dsem], d.dval)
                        else:
                            eng.wait_ge(esem[d.eng], d.sigval)

            @block.tensor
            def _(eng):
                run("tensor", eng)

            @block.vector
            def _(eng):
                run("vector", eng)

            @block.scalar
            def _(eng):
                run("scalar", eng)

            @block.gpsimd
            def _(eng):
                run("gpsimd", eng)

            @block.sync
            def _(eng):
                run("sync", eng)


def alibi_slopes():
    n = 12

    def pow2(m):
        start = 2.0 ** (-8.0 / m)
        return [start ** (i + 1) for i in range(m)]

    c = 2 ** int(math.floor(math.log2(n)))
    s = pow2(c) + (pow2(2 * c)[0::2][: n - c] if c < n else [])
    s = -np.sort(-np.asarray(s, np.float32))
    return s.reshape(4, 3).T


def make_consts():
    bf = ml_dtypes.bfloat16
    C = {}
    C["ident"] = np.eye(128, dtype=np.float32)
    C["identb"] = np.eye(128).astype(bf)
    sl = alibi_slopes().astype(np.float64)
    t = np.arange(SEQ, dtype=np.float64)
    qaug = np.zeros((3, 2, 4, SEQ), np.float32)
    for m in range(3):
        for h in range(4):
            qaug[m, 0, h] = (-sl[m, h] * t).astype(np.float32)
            qaug[m, 1, h] = np.float32(sl[m, h])
    C["qaug"] = qaug
    kaug = np.zeros((2, SEQ), np.float32)
    kaug[0] = 1.0
    kaug[1] = t
    C["kaug"] = kaug
    kaugc = np.zeros((2, 256), np.float32)
    kaugc[0] = 1.0
    kaugc[1] = 16.0 * np.arange(256) + 31.0
    C["kaugc"] = kaugc
    E = np.zeros((64, SEQ), np.float32)
    for j in range(64):
        E[j, j * 64:(j + 1) * 64] = 1.0
    C["E"] = E.astype(bf)
    Em = np.zeros((64, 64, 128), np.float32)
    for v in range(64):
        Em[v, v, :] = 1.0
    C["Em"] = Em.astype(bf)
    cc = np.arange(128)[:, None]
    rr = np.arange(128)[None, :]
    triA = np.where(cc > rr, NEG, 0.0).astype(np.float32)
    triB = np.where(cc <= rr, NEG, 0.0).astype(np.float32)
    C["triA"] = np.tile(triA, (1, 4)).astype(bf)
    C["triB"] = np.tile(triB, (1, 4)).astype(bf)
    t512 = np.zeros((128, 4, 512), np.float32)
    ql = np.arange(512)[None, :]
    for j in range(4):
        t512[:, j, :] = np.where(128 * j + cc > ql, NEG, 0.0)
    C["t512"] = t512.astype(bf)
    mcmp = np.zeros((128, 2, SEQ), np.float32)
    for nt in range(2):
        n = nt * 128 + np.arange(128)[:, None]
        bad = (n >= 255) | (16 * n + 31 > np.arange(SEQ)[None, :])
        mcmp[:, nt, :] = np.where(bad, NEG, 0.0)
    C["mcmp"] = mcmp.astype(bf)
    cur = (np.arange(SEQ) // 64)[:, None]
    j = np.arange(64)[None, :]
    forced = (j == 0) | (j == cur) | (j == cur - 1)
    valid = j <= cur
    selc = np.zeros((SEQ, 3, 64), np.float32)
    selc[:, 0, :] = np.where(forced, 1e4, 0.0)
    selc[:, 1, :] = np.where(valid, 1e30, -1e30)
    selc[:, 2, :] = np.where(valid, 0.0, NEG)
    C["selc"] = selc
    curb = (np.arange(SEQ) // 256)[:, None]
    jb = np.arange(16)[None, :]
    mc = np.zeros((SEQ, 3, 16), np.float32)
    mc[:, 0, :] = np.where(jb < curb, 1e30, -1e30)
    mc[:, 1, :] = np.where(jb < curb, 1.0, 0.0)
    mc[:, 2, :] = np.where(jb == curb, 1.0, 0.0)
    C["mconst"] = mc
    half = 16
    inv = 10000.0 ** (-np.arange(half, dtype=np.float32) / half)
    ang = np.arange(SEQ, dtype=np.float32)[:, None] * inv[None, :]
    C["rope"] = np.concatenate([np.cos(ang), np.sin(ang)], axis=1).astype(np.float32)
    n_cmp = 255
    starts = np.arange(n_cmp) * 16
    jb64 = np.arange(64) * 64
    cover = ((starts[:, None] < jb64[None, :] + 64) & (starts[:, None] + 32 > jb64[None, :])).astype(np.float32)
    cov = np.zeros((256, 64), np.float32)
    cov[:255] = cover
    C["cover"] = np.ascontiguousarray(cov.reshape(2, 128, 64).transpose(1, 0, 2))
    tp = np.arange(128)
    C["U"] = (tp[:, None] < tp[None, :]).astype(np.float32)
    C["bstart"] = np.tile((128.0 * np.arange(NBLK, dtype=np.float32))[None, :], (128, 1)).astype(np.float32)
    C["off13"] = (np.arange(8)[None, :] * 128 + tp[:, None]).astype(np.float32)
    C["off2"] = (np.arange(2)[None, :] * 128 + tp[:, None]).astype(np.float32)
    return C


CONST_SPECS = [
    ("ident", [128, 128], F32), ("identb", [128, 128], BF16), ("qaug", [3, 2, 4, SEQ], F32),
    ("kaug", [2, SEQ], F32), ("kaugc", [2, 256], F32), ("E", [64, SEQ], BF16),
    ("Em", [64, 64, 128], BF16), ("triA", [128, 512], BF16), ("triB", [128, 512], BF16),
    ("t512", [128, 4, 512], BF16), ("mcmp", [128, 2, SEQ], BF16), ("selc", [SEQ, 3, 64], F32),
    ("mconst", [SEQ, 3, 16], F32), ("rope", [SEQ, 32], F32), ("cover", [128, 2, 64], F32),
    ("U", [128, 128], F32), ("bstart", [128, NBLK], F32), ("off13", [128, 8], F32), ("off2", [128, 2], F32),
]

WEIGHT_SPECS = [
    ("w_ada", [D, 6 * D]), ("b_ada", [6 * D]), ("norm_gain", [2, D]), ("w_in", [D, DIN]),
    ("qk_gain", [10, 64]), ("cmp_pe", [2, 32, 64]), ("cmp_w1", [2, 2048, 256]), ("cmp_w2", [2, 256, 64]),
    ("swa_sinks", [4]), ("lat_gain_q", [384]), ("lat_gain_kv", [128]), ("rope_gain", [2, 32]),
    ("w_uq", [384, 384]), ("w_ukv", [128, 512]), ("w_branch", [4, 256, D]), ("w_out", [D, D]),
    ("w_coarse", [D, 4]), ("b_coarse", [4]), ("w_fine", [D, 32]), ("b_fine", [32]),
    ("w13", [32, D, 512]), ("w2", [32, 256, D]),
]


class Builder:
    def __init__(self, n_layers, debug=False, stop_after=None, r32=False):
        self.r32 = r32
        self.nl = n_layers
        self.debug = debug
        self.stop_after = stop_after
        nc = self.nc = bass.Bass("TRN2", target_bir_lowering=False)
        self.S = Sched(nc)
        self.x_in = nc.dram_tensor("x", [SEQ, D], F32, kind="ExternalInput").ap()
        self.c_in = nc.dram_tensor("c", [8, 128], F32, kind="ExternalInput").ap()
        self.Wd = {}
        for name, shp in WEIGHT_SPECS:
            self.Wd[name] = nc.dram_tensor(name, [n_layers] + shp, F32, kind="ExternalInput").ap()
        self.Cd = {}
        for name, shp, dt in CONST_SPECS:
            self.Cd[name] = nc.dram_tensor("k_" + name, shp, dt, kind="ExternalInput").ap()
        self.y = nc.dram_tensor("y", [SEQ, D], F32, kind="ExternalOutput").ap()
        dk = "ExternalOutput" if debug else "Internal"
        self.Pall = nc.dram_tensor("Pall", [SEQ, DIN], F32, kind=dk).ap()
        self.FT = nc.dram_tensor("FT", [NF, SEQ], F32, kind=dk).ap()
        self.Vd = nc.dram_tensor("Vd", [SEQ, 256], F32, kind=dk).ap()
        self.Od = nc.dram_tensor("Od", [SEQ, D], F32, kind=dk).ap()
        self.xmid = nc.dram_tensor("xmid", [SEQ, D], F32, kind=dk).ap()
        self.xpp = [nc.dram_tensor("xpp%d" % i, [SEQ, D], F32, kind="Internal").ap() for i in range(2)]
        self.H2d = nc.dram_tensor("H2d", [SEQ, D], F32, kind="Internal").ap()
        self.Xsort = nc.dram_tensor("Xsort", [NBLK * 128, D], F32, kind="Internal").ap()
        self.Ysort = nc.dram_tensor("Ysort", [NBLK * 128, D], F32, kind="Internal").ap()
        self.outs = []
        if debug:
            self.dbg1 = nc.dram_tensor("dbg1", [SEQ, 256], F32, kind="ExternalOutput").ap()
            self.dbg2 = nc.dram_tensor("dbg2", [SEQ, 128], F32, kind="ExternalOutput").ap()
            self.dbg3 = nc.dram_tensor("dbg3", [128, 2, 64], F32, kind="ExternalOutput").ap()
            self.dbg4 = nc.dram_tensor("dbg4", [128, 2, 129], F32, kind="ExternalOutput").ap()
            self.dbg5 = nc.dram_tensor("dbg5", [66, 256], F32, kind="ExternalOutput").ap()

    def dma(self, out, in_, r=(), w=(), q="sync"):
        return self.S.add(q, lambda e: e.dma_start(out=out, in_=in_), r, w, dma=True)

    def mm(self, out, lhsT, rhs, st, sp, r, w, fast=False):
        if fast and self.r32 and lhsT.dtype == F32 and rhs.dtype == F32:
            lhsT = lhsT.bitcast(F32R)
            rhs = rhs.bitcast(F32R)
        return self.S.add("tensor", lambda e: e.matmul(out, lhsT=lhsT, rhs=rhs, start=st, stop=sp), r, w)

    def tr(self, out, in_, r, w):
        p = in_.shape[0]
        idn = self.ident[0:p, 0:p]
        return self.S.add("tensor", lambda e: e.transpose(out=out, in_=in_, identity=idn), list(r), w)

    def act(self, out, in_, func, r, w, bias=None, scale=None, accum=None):
        kw = {}
        if bias is not None:
            kw["bias"] = bias
        if scale is not None:
            kw["scale"] = scale
        if accum is not None:
            kw["accum_out"] = accum
        return self.S.add("scalar", lambda e: e.activation(out=out, in_=in_, func=func, **kw), r, w)

    def tt(self, out, a, b, op, r, w, eng="vector"):
        return self.S.add(eng, lambda e: e.tensor_tensor(out=out, in0=a, in1=b, op=op), r, w)

    def ts(self, out, a, s1, s2, op0, op1, r, w, eng="vector"):
        if s2 is None:
            return self.S.add(eng, lambda e: e.tensor_scalar(out=out, in0=a, scalar1=s1, scalar2=None, op0=op0), r, w)
        return self.S.add(eng, lambda e: e.tensor_scalar(out=out, in0=a, scalar1=s1, scalar2=s2, op0=op0, op1=op1), r, w)

    def stt(self, out, a, s, b, op0, op1, r, w):
        return self.S.add("vector", lambda e: e.scalar_tensor_tensor(out=out, in0=a, scalar=s, in1=b, op0=op0, op1=op1), r, w)

    def red(self, out, in_, op, r, w):
        return self.S.add("vector", lambda e: e.tensor_reduce(out=out, in_=in_, axis=AX.X, op=op), r, w)

    def cp(self, out, in_, r, w, eng="vector"):
        if eng == "scalar":
            return self.S.add("scalar", lambda e: e.activation(out=out, in_=in_, func=AF.Copy), r, w)
        return self.S.add(eng, lambda e: e.tensor_copy(out=out, in_=in_), r, w)

    def rcp(self, out, in_, r, w):
        return self.S.add("vector", lambda e: e.reciprocal(out=out, in_=in_), r, w)

    def mset(self, ap, val, w):
        return self.S.add("vector", lambda e: e.memset(ap, val), (), w)

    def vmax(self, out, in_, r, w):
        return self.S.add("vector", lambda e: e.max(out=out, in_=in_), r, w)

    def rsq(self, ap, inv_n, key):
        self.ts(ap, ap, inv_n, EPS, ALU.mult, ALU.add, [key], [key])
        self.act(ap, ap, AF.Sqrt, [key], [key])
        self.rcp(ap, ap, [key], [key])

    def reset(self):
        self.S.barrier()
        self.top = PERSIST

    def A(self, n, parts=128):
        n = (n + 1) // 2 * 2
        ap = self.big[0:parts, self.top:self.top + n]
        self.top += n
        assert self.top <= SB_FLOATS, self.top
        return ap

    def bank(self, i):
        return self.ps[:, i * 512:(i + 1) * 512]

    def Wl(self, name, l):
        return self.Wd[name][l]

    def build(self):
        nc = self.nc
        with ExitStack() as es:
            self.big = es.enter_context(nc.sbuf_tensor("big", [128, SB_FLOATS], F32))
            self.ps = es.enter_context(nc.psum_tensor("ps", [128, 4096], F32))
            big = self.big
            self.mod = big[:, 0:6144]
            self.ident = big[:, 6144:6272]
            self.identb = big[:, 6272:6336].bitcast(BF16)
            self.ones = big[:, 6336:6464]
            self.gq = big[:, 6464:7104]
            self.esink = big[:, 7104:7108]
            self.top = PERSIST
            self.dma(self.ident, self.Cd["ident"], (), ["ident"])
            self.dma(self.identb, self.Cd["identb"], (), ["identb"])
            self.mset(self.ones, 1.0, ["ones"])
            for l in range(self.nl):
                xin = self.x_in if l == 0 else self.xpp[(l - 1) % 2]
                xout = self.y if l == self.nl - 1 else self.xpp[l % 2]
                self.layer(l, xin, xout)
            self.S.barrier()
            self.S.emit(final_waits=self.outs)
        return nc

    def layer(self, l, xin, xout):
        st = self.stop_after
        self.ph_mod(l)
        self.ph_inproj(l, xin)
        if st == "inproj":
            return
        self.ph_prep(l)
        if st == "prep":
            return
        self.ph_nsa(l)
        if st == "nsa":
            return
        self.ph_swa(l)
        if st == "swa":
            return
        self.ph_dense(l, moba=True)
        if st == "moba":
            return
        self.ph_dense(l, moba=False)
        if st == "mla":
            return
        self.ph_merge(l, xin)
        if st == "merge":
            return
        if SPARSE_MOE:
            self.ph_moe_sparse(l, xout)
        else:
            self.ph_moe(l, xout)

    def ph_mod(self, l):
        self.reset()
        c8 = self.A(128, 8)
        cT = self.A(8)
        rep = self.A(1024)
        bada = self.A(6144)
        ng = self.A(2048)
        wch = [self.A(4096), self.A(4096)]
        self.dma(c8, self.c_in, (), ["c8"])
        self.tr(self.bank(7)[:, 0:8], c8, ["c8", "ident"], ["ps7"])
        self.act(cT, self.bank(7)[:, 0:8], AF.Silu, ["ps7"], ["cT"])
        for k in range(8):
            self.ts(rep[:, k * 128:(k + 1) * 128], self.ones, cT[:, k:k + 1], None, ALU.mult, None, ["ones", "cT"], ["rep"])
        self.dma(bada, self._bc_row(self.Wd["b_ada"][l:l + 1, :], 6144), (), ["bada"])
        self.dma(ng, self._bc_row(self.Wd["norm_gain"][l:l + 1].rearrange("o a d -> o (a d)"), 2048), (), ["ng"])
        self.dma(self.gq, self._bc_row(self.Wd["qk_gain"][l:l + 1].rearrange("o a d -> o (a d)"), 640), (), ["gq"])
        wa = self.Wl("w_ada", l).rearrange("(kc p) n -> p kc n", p=128)
        for n in range(12):
            b = n % 2
            wv = wch[b].rearrange("p (k n) -> p k n", k=8)
            self.dma(wv, wa[:, :, n * 512:(n + 1) * 512], (), ["wch%d" % b])
            for k in range(8):
                self.mm(self.bank(b), rep[:, k * 128:(k + 1) * 128], wv[:, k, :], k == 0, k == 7, ["rep", "wch%d" % b], ["ps%d" % b])
            self.tt(self.mod[:, n * 512:(n + 1) * 512], self.bank(b), bada[:, n * 512:(n + 1) * 512], ALU.add, ["ps%d" % b, "bada"], ["mod"])
        self.stt(self.mod[:, 1024:2048], self.mod[:, 1024:2048], 1.0, ng[:, 0:1024], ALU.add, ALU.mult, ["mod", "ng"], ["mod"])
        self.stt(self.mod[:, 4096:5120], self.mod[:, 4096:5120], 1.0, ng[:, 1024:2048], ALU.add, ALU.mult, ["mod", "ng"], ["mod"])

    def _bc_row(self, row_ap, n):
        return row_ap.partition_broadcast(128).rearrange("p o n -> p (o n)") if len(row_ap.shape) == 2 else row_ap

    def norm_tile(self, xt, ht, junk, stat, col, a_off, s_off, kx, kh):
        sc = stat[:, col:col + 1]
        self.act(junk, xt, AF.Square, [kx], ["junk", "stat"], accum=sc)
        self.rsq(sc, 1.0 / D, "stat")
        self.stt(ht, xt, sc, self.mod[:, a_off:a_off + D], ALU.mult, ALU.mult, [kx, "stat", "mod"], [kh])
        self.tt(ht, ht, self.mod[:, s_off:s_off + D], ALU.add, [kh, "mod"], [kh])

    def transpose8(self, src, dst3, c0, ksrc, kdst, flip):
        for j in range(2):
            bk = 6 + j
            for kk in range(4):
                self.tr(self.bank(bk)[:, kk * 128:(kk + 1) * 128], src[:, (4 * j + kk) * 128:(4 * j + kk + 1) * 128], [ksrc, "ident"], ["ps%d" % bk])
            self.cp(dst3[:, 4 * j:4 * j + 4, c0:c0 + 128], self.bank(bk).rearrange("p (k t) -> p k t", k=4), ["ps%d" % bk], [kdst],
                    eng=("scalar" if (j + flip) % 2 == 0 else "vector"))

    def ph_inproj(self, l, xin):
        win = self.Wl("w_in", l).rearrange("(kc p) n -> p kc n", p=128)
        for hf in range(2):
            self.reset()
            hT = self.A(16384).rearrange("p (k t) -> p k t", k=8)
            xt = [self.A(1024), self.A(1024)]
            ht = [self.A(1024), self.A(1024)]
            junk = self.A(1024)
            stat = self.A(32)
            wch = [self.A(4096), self.A(4096)]
            stg = [self.A(512) for _ in range(3)]
            for ti in range(16):
                gi = hf * 16 + ti
                b = ti % 2
                self.dma(xt[b], xin[gi * 128:(gi + 1) * 128, :], (), ["xt%d" % b])
                self.norm_tile(xt[b], ht[b], junk, stat, ti, 1024, 0, "xt%d" % b, "ht%d" % b)
                self.transpose8(ht[b], hT, ti * 128, "ht%d" % b, "hT", ti)
            cnt = 0
            for cc in range(13):
                ncol = min(512, DIN - cc * 512)
                b = cc % 2
                wv = wch[b].rearrange("p (k n) -> p k n", k=8)[:, :, 0:ncol]
                self.dma(wv, win[:, :, cc * 512:cc * 512 + ncol], (), ["wch%d" % b])
                for ti in range(16):
                    gi = hf * 16 + ti
                    bk = cnt % 4
                    sg = cnt % 3
                    for k in range(8):
                        self.mm(self.bank(bk)[:, 0:ncol], hT[:, k, ti * 128:(ti + 1) * 128], wv[:, k, :], k == 0, k == 7, ["hT", "wch%d" % b], ["ps%d" % bk], fast=True)
                    self.cp(stg[sg][:, 0:ncol], self.bank(bk)[:, 0:ncol], ["ps%d" % bk], ["stg%d" % sg], eng=("scalar" if cnt % 2 == 0 else "vector"))
                    self.dma(self.Pall[gi * 128:(gi + 1) * 128, cc * 512:cc * 512 + ncol], stg[sg][:, 0:ncol], ["stg%d" % sg], ["Pall"], q="gpsimd")
                    cnt += 1

    def ph_prep(self, l):
        self.reset()
        gq = self.gq
        gainA = self.A(640)
        gainB = self.A(1024)
        lgq = self.A(384)
        lgkv = self.A(128)
        rg = self.A(64)
        gqd = self.A(192)
        wuq = self.A(3 * 384).rearrange("p (k n) -> p k n", k=3)
        wukv = self.A(512)
        Pt = [self.A(NQKV), self.A(NQKV)]
        cs = [self.A(32), self.A(32)]
        sq = self.A(1676)
        ZA = self.A(640)
        ZB = self.A(1024)
        cqn = self.A(512)
        cT = self.A(512).rearrange("p (k t) -> p k t", k=4)
        qf = self.A(512).rearrange("p (h d) -> p h d", h=4)
        kf = self.A(512).rearrange("p (h d) -> p h d", h=4)
        sqq = self.A(384)
        sqk = self.A(256)
        qr = self.A(128)
        krn = self.A(32)
        krot = self.A(32)
        tmp = [self.A(64) for _ in range(4)]
        vd = self.A(256)
        stat = self.A(64)
        trs = [self.A(512) for _ in range(3)]
        junk = self.A(384)
        self.dma(lgq, self._bc_row(self.Wd["lat_gain_q"][l:l + 1, :], 384), (), ["lg"])
        self.dma(lgkv, self._bc_row(self.Wd["lat_gain_kv"][l:l + 1, :], 128), (), ["lg"])
        self.dma(rg, self._bc_row(self.Wd["rope_gain"][l:l + 1].rearrange("o a d -> o (a d)"), 64), (), ["lg"])
        self.dma(wuq, self.Wl("w_uq", l).rearrange("(k p) n -> p k n", p=128), (), ["wu"])
        self.dma(wukv, self.Wl("w_ukv", l), (), ["wu"])
        self.mset(gainA, 1.0, ["gain"])
        self.mset(gainB, 1.0, ["gain"])

        def gset(dst, g0, ng_, slot, scale):
            o = dst[:, g0 * 64:(g0 + ng_) * 64].rearrange("p (g d) -> p g d", d=64)
            i = gq[:, slot * 64:(slot + 1) * 64].unsqueeze(1).to_broadcast([128, ng_, 64])
            self.ts(o, i, scale, None, ALU.mult, None, ["gq", "gain"], ["gain"])

        gset(gainA, 0, 4, 0, 0.125)
        gset(gainA, 6, 1, 2, 1.0)
        gset(gainA, 8, 1, 3, 1.0)
        gset(gainB, 0, 4, 4, 0.125)
        gset(gainB, 4, 2, 5, 1.0)
        gset(gainB, 8, 4, 6, 0.125)
        gset(gainB, 12, 4, 7, 1.0)
        sd = 96.0 ** -0.5
        self.ts(gqd[:, 0:64], gq[:, 512:576], sd, None, ALU.mult, None, ["gq", "lg"], ["gqd"])
        self.ts(gqd[:, 64:96], rg[:, 0:32], sd, None, ALU.mult, None, ["gq", "lg"], ["gqd"])
        self.ts(gqd[:, 96:160], gq[:, 576:640], 1.0, None, ALU.mult, None, ["gq", "lg"], ["gqd"])
        self.ts(gqd[:, 160:192], rg[:, 32:64], 1.0, None, ALU.mult, None, ["gq", "lg"], ["gqd"])
        self.mset(qf.rearrange("p h d -> p (h d)"), 0.0, ["qf"])
        self.mset(kf.rearrange("p h d -> p (h d)"), 0.0, ["kf"])
        tcnt = [0]

        def tgroup(blocks, row0, c0):
            g = tcnt[0]
            tcnt[0] += 1
            bk = g % 4
            sg = g % 3
            for i, (ap, key) in enumerate(blocks):
                self.tr(self.bank(bk)[:, i * 128:(i + 1) * 128], ap, [key, "ident"], ["ps%d" % bk])
            self.cp(trs[sg], self.bank(bk), ["ps%d" % bk], ["trs%d" % sg], eng=("scalar" if g % 2 == 0 else "vector"))
            self.dma(self.FT[row0:row0 + 512, c0:c0 + 128].rearrange("(b p) t -> p b t", p=128),
                     trs[sg].rearrange("p (b t) -> p b t", b=4), ["trs%d" % sg], ["FT"], q="gpsimd")

        for gi in range(NT):
            b = gi % 2
            P = Pt[b]
            kP = "Pt%d" % b
            c0 = gi * 128
            self.dma(P, self.Pall[c0:c0 + 128, 0:NQKV], ["Pall"], [kP])
            self.dma(cs[b], self.Cd["rope"][c0:c0 + 128, :], (), ["cs%d" % b])
            self.act(sq, P[:, 0:1676], AF.Square, [kP], ["sq"])
            self.red(stat[:, 0:10], sq[:, 0:640].rearrange("p (g d) -> p g d", d=64), ALU.add, ["sq"], ["stat"])
            self.red(stat[:, 10:26], sq[:, 652:1676].rearrange("p (g d) -> p g d", d=64), ALU.add, ["sq"], ["stat"])
            self.rsq(stat[:, 0:26], 1.0 / 64, "stat")
            self.mset(stat[:, 4:6], 1.0, ["stat"])
            self.tt(ZA.rearrange("p (g d) -> p g d", d=64), P[:, 0:640].rearrange("p (g d) -> p g d", d=64),
                    stat[:, 0:10].unsqueeze(2).to_broadcast([128, 10, 64]), ALU.mult, [kP, "stat"], ["ZA"])
            self.tt(ZA, ZA, gainA, ALU.mult, ["ZA", "gain"], ["ZA"])
            self.tt(ZB.rearrange("p (g d) -> p g d", d=64), P[:, 652:1676].rearrange("p (g d) -> p g d", d=64),
                    stat[:, 10:26].unsqueeze(2).to_broadcast([128, 16, 64]), ALU.mult, [kP, "stat"], ["ZB"])
            self.tt(ZB, ZB, gainB, ALU.mult, ["ZB", "gain"], ["ZB"])
            tgroup([(ZA[:, i * 128:(i + 1) * 128], "ZA") for i in range(4)], 0, c0)
            tgroup([(ZA[:, 512:640], "ZA")] + [(ZB[:, i * 128:(i + 1) * 128], "ZB") for i in range(3)], 512, c0)
            tgroup([(ZB[:, i * 128:(i + 1) * 128], "ZB") for i in range(4, 8)], 1152, c0)
            self.act(junk, P[:, 1932:2316], AF.Square, [kP], ["junk", "st2"], accum=stat[:, 32:33])
            self.act(junk[:, 0:128], P[:, 2316:2444], AF.Square, [kP], ["junk", "st2"], accum=stat[:, 33:34])
            self.act(junk[:, 0:32], P[:, 2444:2476], AF.Square, [kP], ["junk", "st2"], accum=stat[:, 34:35])
            self.ts(stat[:, 32:33], stat[:, 32:33], 1.0 / 384, EPS, ALU.mult, ALU.add, ["st2"], ["st2"])
            self.ts(stat[:, 33:34], stat[:, 33:34], 1.0 / 128, EPS, ALU.mult, ALU.add, ["st2"], ["st2"])
            self.ts(stat[:, 34:35], stat[:, 34:35], 1.0 / 32, EPS, ALU.mult, ALU.add, ["st2"], ["st2"])
            self.act(stat[:, 32:35], stat[:, 32:35], AF.Sqrt, ["st2"], ["st2"])
            self.rcp(stat[:, 32:35], stat[:, 32:35], ["st2"], ["st2"])
            self.stt(cqn[:, 0:384], P[:, 1932:2316], stat[:, 32:33], lgq, ALU.mult, ALU.mult, [kP, "st2", "lg"], ["cqn"])
            self.stt(cqn[:, 384:512], P[:, 2316:2444], stat[:, 33:34], lgkv, ALU.mult, ALU.mult, [kP, "st2", "lg"], ["cqn"])
            self.stt(krn, P[:, 2444:2476], stat[:, 34:35], gqd[:, 160:192], ALU.mult, ALU.mult, [kP, "st2", "gqd"], ["krn"])
            for i in range(4):
                self.tr(self.bank(4)[:, i * 128:(i + 1) * 128], cqn[:, i * 128:(i + 1) * 128], ["cqn", "ident"], ["ps4"])
            self.cp(cT.rearrange("p k t -> p (k t)"), self.bank(4), ["ps4"], ["cT"], eng="scalar")
            for k in range(3):
                self.mm(self.bank(5)[:, 0:384], cT[:, k, :], wuq[:, k, :], k == 0, k == 2, ["cT", "wu"], ["ps5"])
            self.mm(self.bank(6), cT[:, 3, :], wukv, True, True, ["cT", "wu"], ["ps6"])
            qup = self.bank(5)[:, 0:384].rearrange("p (h d) -> p h d", h=4)
            kvup = self.bank(6).rearrange("p (h d) -> p h d", h=4)
            self.act(sqq, self.bank(5)[:, 0:384], AF.Square, ["ps5"], ["sqq"])
            self.act(sqk.rearrange("p (h d) -> p h d", h=4), kvup[:, :, 0:64], AF.Square, ["ps6"], ["sqk"])
            sq3 = sqq.rearrange("p (h d) -> p h d", h=4)
            self.red(stat[:, 40:44], sq3[:, :, 0:64], ALU.add, ["sqq"], ["st3"])
            self.red(stat[:, 44:48], sqk.rearrange("p (h d) -> p h d", h=4), ALU.add, ["sqk"], ["st3"])
            self.red(stat[:, 48:52], sq3[:, :, 64:96], ALU.add, ["sqq"], ["st3"])
            self.ts(stat[:, 40:48], stat[:, 40:48], 1.0 / 64, EPS, ALU.mult, ALU.add, ["st3"], ["st3"])
            self.ts(stat[:, 48:52], stat[:, 48:52], 1.0 / 32, EPS, ALU.mult, ALU.add, ["st3"], ["st3"])
            self.act(stat[:, 40:52], stat[:, 40:52], AF.Sqrt, ["st3"], ["st3"])
            self.rcp(stat[:, 40:52], stat[:, 40:52], ["st3"], ["st3"])
            self.tt(qf[:, :, 0:64], qup[:, :, 0:64], stat[:, 40:44].unsqueeze(2).to_broadcast([128, 4, 64]), ALU.mult, ["ps5", "st3"], ["qf"])
            self.tt(qf[:, :, 0:64], qf[:, :, 0:64], gqd[:, 0:64].unsqueeze(1).to_broadcast([128, 4, 64]), ALU.mult, ["qf", "gqd"], ["qf"])
            self.tt(kf[:, :, 0:64], kvup[:, :, 0:64], stat[:, 44:48].unsqueeze(2).to_broadcast([128, 4, 64]), ALU.mult, ["ps6", "st3"], ["kf"])
            self.tt(kf[:, :, 0:64], kf[:, :, 0:64], gqd[:, 96:160].unsqueeze(1).to_broadcast([128, 4, 64]), ALU.mult, ["kf", "gqd"], ["kf"])
            self.cp(vd.rearrange("p (h d) -> p h d", h=4), kvup[:, :, 64:128], ["ps6"], ["vd"], eng="scalar")
            self.dma(self.Vd[c0:c0 + 128, :], vd, ["vd"], ["Vd"], q="gpsimd")
            q3 = qr.rearrange("p (h d) -> p h d", h=4)
            self.tt(q3, qup[:, :, 64:96], stat[:, 48:52].unsqueeze(2).to_broadcast([128, 4, 32]), ALU.mult, ["ps5", "st3"], ["qr"])
            self.tt(q3, q3, gqd[:, 64:96].unsqueeze(1).to_broadcast([128, 4, 32]), ALU.mult, ["qr", "gqd"], ["qr"])
            cosb = cs[b][:, 0:16].unsqueeze(1).to_broadcast([128, 4, 16])
            sinb = cs[b][:, 16:32].unsqueeze(1).to_broadcast([128, 4, 16])
            t0 = tmp[0].rearrange("p (h d) -> p h d", h=4)
            t1 = tmp[1].rearrange("p (h d) -> p h d", h=4)
            kc = "cs%d" % b
            self.tt(t0, q3[:, :, 0:16], cosb, ALU.mult, ["qr", kc], ["t0"])
            self.tt(t1, q3[:, :, 16:32], sinb, ALU.mult, ["qr", kc], ["t1"])
            self.tt(qf[:, :, 64:80], t0, t1, ALU.subtract, ["t0", "t1"], ["qf"])
            self.tt(t0, q3[:, :, 0:16], sinb, ALU.mult, ["qr", kc], ["t0"])
            self.tt(t1, q3[:, :, 16:32], cosb, ALU.mult, ["qr", kc], ["t1"])
            self.tt(qf[:, :, 80:96], t0, t1, ALU.add, ["t0", "t1"], ["qf"])
            c1 = cs[b][:, 0:16]
            s1 = cs[b][:, 16:32]
            self.tt(tmp[2][:, 0:16], krn[:, 0:16], c1, ALU.mult, ["krn", kc], ["t2"])
            self.tt(tmp[3][:, 0:16], krn[:, 16:32], s1, ALU.mult, ["krn", kc], ["t3"])
            self.tt(krot[:, 0:16], tmp[2][:, 0:16], tmp[3][:, 0:16], ALU.subtract, ["t2", "t3"], ["krot"])
            self.tt(tmp[2][:, 0:16], krn[:, 0:16], s1, ALU.mult, ["krn", kc], ["t2"])
            self.tt(tmp[3][:, 0:16], krn[:, 16:32], c1, ALU.mult, ["krn", kc], ["t3"])
            self.tt(krot[:, 16:32], tmp[2][:, 0:16], tmp[3][:, 0:16], ALU.add, ["t2", "t3"], ["krot"])
            self.cp(kf[:, :, 64:96], krot.unsqueeze(1).to_broadcast([128, 4, 32]), ["krot"], ["kf"])
            tgroup([(qf[:, h, :], "qf") for h in range(4)], 1664, c0)
            tgroup([(kf[:, h, :], "kf") for h in range(4)], 2176, c0)

    def load_v(self, V, src_ap, nh, key):
        V4 = V.rearrange("p (k h d) -> p k h d", k=32, h=nh)
        self.mset(V, 1.0, [key])
        for c in range(4):
            for h in range(nh):
                self.dma(V4[:, c * 8:(c + 1) * 8, h, 0:64],
                         src_ap[c * 1024:(c + 1) * 1024, h * 64:(h + 1) * 64].rearrange("(k p) d -> p k d", p=128), ["Pall", "Vd"], [key])
        return V4

    def ph_nsa(self, l):
        self.reset()
        FT = self.FT
        raw = self.A(4096)
        w1c = self.A(8192).rearrange("p (r m) -> p r m", r=32)
        pe32 = self.A(128, 32)
        peT = self.A(32)
        cpe = self.A(4)
        hid = [[self.A(256) for _ in range(2)] for _ in range(2)]
        w2c = self.A(256).rearrange("p (s c d) -> p s c d", s=2, c=2)
        kcn = self.A(128).rearrange("p (c d) -> p c d", c=2)
        sqc = self.A(128)
        kcT = self.A(256)
        vcx = self.A(258).rearrange("p (c d) -> p c d", c=2)
        KsT = self.A(4096)
        KwT = self.A(4096)
        Vs = self.A(32 * 65)
        Vw = self.A(32 * 65)
        Ebf = self.A(2048, 64).bitcast(BF16)
        mcmp = self.A(4096).bitcast(BF16).rearrange("p (c t) -> p c t", c=2)
        triA = self.A(256).bitcast(BF16)
        triB = self.A(256).bitcast(BF16)
        qa = [self.A(512), self.A(512)]
        gl = [self.A(12), self.A(12)]
        selc = [self.A(192), self.A(192)]
        PT = [self.A(512) for _ in range(3)]
        oa = self.A(256)
        stat = self.A(64)
        score = self.A(64)
        top8 = self.A(8)
        selb = self.A(64)
        selbT = self.A(256, 64).bitcast(BF16)
        otsb = self.A(512)
        atsb = self.A(512)
        Cd = self.Cd
        self.dma(Ebf, Cd["E"], (), ["Ebf"])
        self.dma(mcmp, Cd["mcmp"], (), ["mcmp"])
        self.dma(triA, Cd["triA"], (), ["tri"])
        self.dma(triB, Cd["triB"], (), ["tri"])
        self.dma(raw, FT[256:384, :], ["FT"], ["raw"])
        for s in range(2):
            self.dma(w1c[64 * s:64 * s + 64], self.Wd["cmp_w1"][l, s].rearrange("(r d) m -> d r m", d=64), (), ["w1c"])
            self.dma(pe32[:, 64 * s:64 * s + 64], self.Wd["cmp_pe"][l, s], (), ["pe32"])
        self.dma(w2c, self.Wd["cmp_w2"][l].rearrange("s (c p) d -> p s c d", p=128), (), ["w2c"])
        self.dma(KsT[0:64], FT[384:448, :], ["FT"], ["KsT"])
        self.dma(KsT[64:66], Cd["kaug"], (), ["KsT"])
        self.dma(KwT[0:64], FT[512:576, :], ["FT"], ["KwT"])
        self.dma(KwT[64:66], Cd["kaug"], (), ["KwT"])
        Vs4 = self.load_v(Vs, self.Pall[:, 448:512], 1, "Vs")
        Vw4 = self.load_v(Vw, self.Pall[:, 576:640], 1, "Vw")
        self.tr(self.bank(7)[:, 0:32], pe32, ["pe32", "ident"], ["ps7"])
        self.cp(peT, self.bank(7)[:, 0:32], ["ps7"], ["peT"])
        for s in range(2):
            sp = slice(64 * s, 64 * s + 64)
            for mc in range(2):
                col = s * 2 + mc
                for r in range(32):
                    self.mm(self.bank(4 + col)[:, 0:1], w1c[sp, r, mc * 128:(mc + 1) * 128], peT[sp, r:r + 1], r == 0, r == 31, ["w1c", "peT"], ["ps%d" % (4 + col)])
                self.cp(cpe[:, col:col + 1], self.bank(4 + col)[:, 0:1], ["ps%d" % (4 + col)], ["cpe"])
        for s in range(2):
            sp = slice(64 * s, 64 * s + 64)
            for mc in range(2):
                bk = (s * 2 + mc) % 2
                for r in range(32):
                    self.mm(self.bank(bk)[:, 0:255], w1c[sp, r, mc * 128:(mc + 1) * 128], raw[sp, r:r + 16 * 254 + 1:16], r == 0, r == 31, ["w1c", "raw"], ["ps%d" % bk])
                hk = "hid%d%d" % (s, mc)
                self.mset(hid[s][mc], 0.0, [hk])
                self.act(hid[s][mc][:, 0:255], self.bank(bk)[:, 0:255], AF.Silu, ["ps%d" % bk, "cpe"], [hk], bias=cpe[:, s * 2 + mc:s * 2 + mc + 1])
        self.mset(vcx.rearrange("p c d -> p (c d)"), 1.0, ["vcx"])
        self.dma(vcx[:, :, 65:129], Cd["cover"], (), ["vcx"])
        for s in range(2):
            for ch in range(2):
                bk = 2 + ch
                for mc in range(2):
                    self.mm(self.bank(bk)[:, 0:64], hid[s][mc][:, ch * 128:(ch + 1) * 128], w2c[:, s, mc, :], mc == 0, mc == 1,
                            ["hid%d%d" % (s, mc), "w2c"], ["ps%d" % bk])
                if s == 0:
                    self.cp(kcn[:, ch, :], self.bank(bk)[:, 0:64], ["ps%d" % bk], ["kcn"])
                else:
                    self.cp(vcx[:, ch, 0:64], self.bank(bk)[:, 0:64], ["ps%d" % bk], ["vcx"])
        kc2 = kcn.rearrange("p c d -> p (c d)")
        self.tt(sqc, kc2, kc2, ALU.mult, ["kcn"], ["sqc"])
        self.red(stat[:, 0:2], sqc.rearrange("p (c d) -> p c d", c=2), ALU.add, ["sqc"], ["stat"])
        self.rsq(stat[:, 0:2], 1.0 / 64, "stat")
        self.tt(kcn, kcn, stat[:, 0:2].unsqueeze(2).to_broadcast([128, 2, 64]), ALU.mult, ["kcn", "stat"], ["kcn"])
        self.tt(kcn, kcn, self.gq[:, 64:128].unsqueeze(1).to_broadcast([128, 2, 64]), ALU.mult, ["kcn", "gq"], ["kcn"])
        for ch in range(2):
            self.tr(self.bank(7)[0:64, ch * 128:(ch + 1) * 128], kcn[:, ch, :], ["kcn", "ident"], ["ps7"])
        self.cp(kcT[0:64], self.bank(7)[0:64, 0:256], ["ps7"], ["kcT"])
        self.dma(kcT[64:66], Cd["kaugc"], (), ["kcT"])
        identb = self.identb
        if self.debug:
            self.dma(self.dbg3, kcn, ["kcn"], ["dbg3"], q="gpsimd")
            self.dma(self.dbg4, vcx, ["vcx"], ["dbg4"], q="gpsimd")
            self.dma(self.dbg5, kcT[0:66], ["kcT"], ["dbg5"], q="gpsimd")
        for i in range(NT):
            b = i % 2
            c0 = i * 128
            q = qa[b]
            kq = "qa%d" % b
            q3 = q.rearrange("p (h t) -> p h t", h=4)
            self.dma(q3[0:64], FT[0:256, c0:c0 + 128].rearrange("(h d) t -> d h t", d=64), ["FT"], [kq])
            self.dma(q3[64:66], Cd["qaug"][0][:, :, c0:c0 + 128], (), [kq])
            self.dma(gl[b], self.Pall[c0:c0 + 128, 640:652], ["Pall"], ["gl%d" % b])
            self.dma(selc[b], Cd["selc"][c0:c0 + 128].rearrange("p a j -> p (a j)"), (), ["selc%d" % b])
            g12 = gl[b]
            self.act(g12, g12, AF.Sigmoid, ["gl%d" % b], ["gl%d" % b])
            g3 = g12.rearrange("p (h k) -> p h k", k=3)
            nnt = 1 if i < 16 else 2
            for nt in range(nnt):
                self.mm(self.bank(nt), kcT[0:66, nt * 128:(nt + 1) * 128], q[0:66, :], True, False, ["kcT", kq], ["ps%d" % nt])
                self.mm(self.bank(nt).rearrange("p (h t) -> p h t", h=4), identb, mcmp[:, nt, c0:c0 + 128].unsqueeze(1).to_broadcast([128, 4, 128]),
                        False, True, ["identb", "mcmp"], ["ps%d" % nt])
                self.act(PT[nt], self.bank(nt), AF.Exp, ["ps%d" % nt], ["PT%d" % nt])
            for nt in range(nnt):
                self.mm(self.bank(2)[0:65, :], vcx[:, nt, 0:65], PT[nt], nt == 0, nt == nnt - 1, ["PT%d" % nt, "vcx"], ["ps2"])
            for nt in range(nnt):
                self.mm(self.bank(3)[0:64, :], vcx[:, nt, 65:129], PT[nt], nt == 0, nt == nnt - 1, ["PT%d" % nt, "vcx"], ["ps3"])
            self.cp(otsb[0:65, :], self.bank(2)[0:65, :], ["ps2"], ["otsb"], eng="scalar")
            self.cp(atsb[0:64, :], self.bank(3)[0:64, :], ["ps3"], ["atsb"])
            for h in range(4):
                self.tr(self.bank(4)[:, h * 65:(h + 1) * 65], otsb[0:65, h * 128:(h + 1) * 128], ["otsb", "ident"], ["ps4"])
                self.tr(self.bank(5)[:, h * 64:(h + 1) * 64], atsb[0:64, h * 128:(h + 1) * 128], ["atsb", "ident"], ["ps5"])

            def creg(h):
                return self.bank(4)[:, h * 65:(h + 1) * 65]

            def areg(h):
                return self.bank(5)[:, h * 64:(h + 1) * 64]

            for h in range(4):
                self.ts(stat[:, 8 + h:9 + h], creg(h)[:, 64:65], 1e-30, None, ALU.max, None, ["ps4"], ["st"])
            self.rcp(stat[:, 8:12], stat[:, 8:12], ["st"], ["st"])
            self.tt(stat[:, 12:16], stat[:, 8:12], g3[:, :, 0], ALU.mult, ["st", "gl%d" % b], ["st"])
            oa3 = oa.rearrange("p (h d) -> p h d", h=4)
            for h in range(4):
                self.ts(oa3[:, h, :], creg(h)[:, 0:64], stat[:, 12 + h:13 + h], None, ALU.mult, None, ["ps4", "st"], ["oa"])
            self.ts(score, areg(0), stat[:, 8:9], None, ALU.mult, None, ["ps5", "st"], ["score"])
            for h in range(1, 4):
                self.stt(score, areg(h), stat[:, 8 + h:9 + h], score, ALU.mult, ALU.add, ["ps5", "st", "score"], ["score"])
            sc3 = selc[b].rearrange("p (a j) -> p a j", a=3)
            ksc = "selc%d" % b
            self.tt(score, score, sc3[:, 0, :], ALU.add, ["score", ksc], ["score"])
            self.tt(score, score, sc3[:, 1, :], ALU.min, ["score", ksc], ["score"])
            self.vmax(top8, score, ["score"], ["top8"])
            self.ts(selb, score, top8[:, 7:8], None, ALU.is_ge, None, ["score", "top8"], ["selb"])
            self.ts(selb, selb, -NEG, NEG, ALU.mult, ALU.add, ["selb"], ["selb"])
            self.tt(selb, selb, sc3[:, 2, :], ALU.add, ["selb", ksc], ["selb"])
            if self.debug:
                self.dma(self.dbg1[c0:c0 + 128, :], oa, ["oa"], ["dbg1"], q="gpsimd")
                self.dma(self.dbg2[c0:c0 + 128, 0:64], score, ["score"], ["dbg2"], q="gpsimd")
                self.dma(self.dbg2[c0:c0 + 128, 64:128], selb, ["selb"], ["dbg2"], q="gpsimd")
            self.tr(self.bank(6)[0:64, 0:128], selb, ["selb", "ident"], ["ps6"])
            self.cp(selbT.rearrange("p (h t) -> p h t", h=4), self.bank(6)[0:64, 0:128].unsqueeze(1).to_broadcast([64, 4, 128]), ["ps6"], ["selbT"])
            for br in (1, 2):
                kts = list(range(0, i + 1)) if br == 1 else list(range(max(0, i - 4), i + 1))
                KT = KsT if br == 1 else KwT
                kk = "KsT" if br == 1 else "KwT"
                V4 = Vs4 if br == 1 else Vw4
                kv = "Vs" if br == 1 else "Vw"
                nk_ = len(kts)
                for n_ in range(nk_ + 1):
                    if n_ < nk_:
                        kt = kts[n_]
                        bk = n_ % 2
                        pt = n_ % 3
                        pk = "ps%d" % bk
                        last_extra = (kt == i) or (br == 2 and kt == i - 4)
                        self.mm(self.bank(bk), KT[0:66, kt * 128:(kt + 1) * 128], q[0:66, :], True, (br == 2 and not last_extra), [kk, kq], [pk])
                        if br == 1:
                            self.mm(self.bank(bk), Ebf[:, kt * 128:(kt + 1) * 128], selbT, False, not last_extra, ["Ebf", "selbT"], [pk])
                        if kt == i:
                            self.mm(self.bank(bk), identb, triA, False, True, ["identb", "tri"], [pk])
                        elif br == 2 and kt == i - 4:
                            self.mm(self.bank(bk), identb, triB, False, True, ["identb", "tri"], [pk])
                        self.act(PT[pt], self.bank(bk), AF.Exp, [pk], ["PT%d" % pt])
                    if n_ >= 1:
                        m_ = n_ - 1
                        self.mm(self.bank(2)[0:65, :], V4[:, kts[m_], 0, :], PT[m_ % 3], m_ == 0, m_ == nk_ - 1, ["PT%d" % (m_ % 3), kv], ["ps2"])
                self.cp(otsb[0:65, :], self.bank(2)[0:65, :], ["ps2"], ["otsb"], eng="scalar")
                for h in range(4):
                    self.tr(self.bank(4)[:, h * 65:(h + 1) * 65], otsb[0:65, h * 128:(h + 1) * 128], ["otsb", "ident"], ["ps4"])
                for h in range(4):
                    self.cp(stat[:, 16 + h:17 + h], creg(h)[:, 64:65], ["ps4"], ["st"])
                self.rcp(stat[:, 16:20], stat[:, 16:20], ["st"], ["st"])
                self.tt(stat[:, 20:24], stat[:, 16:20], g3[:, :, br], ALU.mult, ["st", "gl%d" % b], ["st"])
                for h in range(4):
                    self.stt(oa3[:, h, :], creg(h)[:, 0:64], stat[:, 20 + h:21 + h], oa3[:, h, :], ALU.mult, ALU.add,
                             ["ps4", "st", "oa"], ["oa"])
            self.dma(self.Od[c0:c0 + 128, 0:256], oa, ["oa"], ["Od"], q="gpsimd")

    def ph_swa(self, l):
        self.reset()
        FT = self.FT
        Cd = self.Cd
        KTb = self.A(2 * 4096).rearrange("p (g t) -> p g t", g=2)
        Vb = self.A(32 * 2 * 65)
        triA = self.A(256).bitcast(BF16)
        triB = self.A(256).bitcast(BF16)
        qb = [self.A(512), self.A(512)]
        PT = [self.A(512) for _ in range(3)]
        ob = self.A(256)
        stat = self.A(16)
        otsb = self.A(512)
        identb = self.identb
        self.dma(triA, Cd["triA"], (), ["tri"])
        self.dma(triB, Cd["triB"], (), ["tri"])
        self.dma(KTb[0:64], FT[896:1024, :].rearrange("(g d) t -> d g t", d=64), ["FT"], ["KTb"])
        for g in range(2):
            self.dma(KTb[64:66, g, :], Cd["kaug"], (), ["KTb"])
        Vb4 = self.load_v(Vb, self.Pall[:, 1036:1164], 2, "Vb")
        self.dma(self.esink, self._bc_row(self.Wd["swa_sinks"][l:l + 1, :], 4), (), ["esink"])
        self.act(self.esink, self.esink, AF.Exp, ["esink"], ["esink"])
        ob3 = ob.rearrange("p (h d) -> p h d", h=4)
        for i in range(NT):
            b = i % 2
            c0 = i * 128
            q = qb[b]
            kq = "qb%d" % b
            q3 = q.rearrange("p (h t) -> p h t", h=4)
            self.dma(q3[0:64], FT[640:896, c0:c0 + 128].rearrange("(h d) t -> d h t", d=64), ["FT"], [kq])
            self.dma(q3[64:66], Cd["qaug"][1][:, :, c0:c0 + 128], (), [kq])
            kts = [kt for kt in (i - 1, i) if kt >= 0]
            for n_, kt in enumerate(kts):
                bk = (i + n_) % 2
                pt = (i + n_) % 3
                pk = "ps%d" % bk
                tri = triA if kt == i else triB
                for g in range(2):
                    reg = self.bank(bk)[:, g * 256:(g + 1) * 256]
                    self.mm(reg, KTb[0:66, g, kt * 128:(kt + 1) * 128], q[0:66, g * 256:(g + 1) * 256], True, False, ["KTb", kq], [pk])
                    self.mm(reg, identb, tri[:, 0:256], False, True, ["identb", "tri"], [pk])
                self.act(PT[pt], self.bank(bk), AF.Exp, [pk], ["PT%d" % pt])
                for g in range(2):
                    self.mm(self.bank(2 + g)[0:65, 0:256], Vb4[:, kt, g, :], PT[pt][:, g * 256:(g + 1) * 256], n_ == 0, n_ == len(kts) - 1,
                            ["PT%d" % pt, "Vb"], ["ps%d" % (2 + g)])
            for g in range(2):
                self.cp(otsb[0:65, g * 256:(g + 1) * 256], self.bank(2 + g)[0:65, 0:256], ["ps%d" % (2 + g)], ["otsb"], eng=("scalar" if g == 0 else "vector"))
            for h in range(4):
                self.tr(self.bank(4)[:, h * 65:(h + 1) * 65], otsb[0:65, h * 128:(h + 1) * 128], ["otsb", "ident"], ["ps4"])
            for h in range(4):
                self.tt(stat[:, h:h + 1], self.bank(4)[:, h * 65 + 64:h * 65 + 65], self.esink[:, h:h + 1], ALU.add, ["ps4", "esink"], ["st"])
            self.rcp(stat[:, 0:4], stat[:, 0:4], ["st"], ["st"])
            for h in range(4):
                self.ts(ob3[:, h, :], self.bank(4)[:, h * 65:h * 65 + 64], stat[:, h:h + 1], None, ALU.mult, None, ["ps4", "st"], ["ob"])
            self.dma(self.Od[c0:c0 + 128, 256:512], ob, ["ob"], ["Od"], q="gpsimd")

    def ph_dense(self, l, moba):
        self.reset()
        FT = self.FT
        Cd = self.Cd
        KR = 66 if moba else 96
        KT = self.A(4 * 4096).rearrange("p (h t) -> p h t", h=4)
        V = self.A(32 * 4 * 65)
        t512 = self.A(1024).bitcast(BF16).rearrange("p (j t) -> p j t", j=4)
        qg = [self.A(2048), self.A(2048)]
        PT = [self.A(512) for _ in range(3)]
        oc = self.A(1024).rearrange("p (j h d) -> p j h d", j=4, h=4)
        stat = self.A(16)
        otsb = [self.A(512), self.A(512)]
        cnt = 0
        ptl = []
        identb = self.identb
        self.dma(t512, Cd["t512"], (), ["t512"])
        if moba:
            Embf = self.A(4096, 64).bitcast(BF16).rearrange("p (v k) -> p v k", v=64)
            kmT = self.A(64, 64).rearrange("p (h j) -> p h j", h=4)
            mcst = [self.A(48), self.A(48)]
            gsm = self.A(64)
            t8 = self.A(32)
            selb = self.A(64)
            selbT = self.A(256, 64).bitcast(BF16)
            self.dma(Embf, Cd["Em"], (), ["Embf"])
            self.dma(KT[0:64], FT[1408:1664, :].rearrange("(h d) t -> d h t", d=64), ["FT"], ["KT"])
            for h in range(4):
                self.dma(KT[64:66, h, :], Cd["kaug"], (), ["KT"])
            V4 = self.load_v(V, self.Pall[:, 1676:1932], 4, "V")
            self.red(kmT, KT[0:64].rearrange("p h (j t) -> p h j t", j=16), ALU.add, ["KT"], ["kmT"])
            ocol = 512
            qrow = 1152
        else:
            self.dma(KT[0:96], FT[2176:2688, :].rearrange("(h r) t -> r h t", r=128)[0:96], ["FT"], ["KT"])
            V4 = self.load_v(V, self.Vd, 4, "V")
            ocol = 768
            qrow = 1664
        for g in range(8):
            b = g % 2
            q = qg[b]
            kq = "qg%d" % b
            q3 = q.rearrange("p (h t) -> p h t", h=4)
            g0 = g * 512
            if moba:
                self.dma(q3[0:64], FT[qrow:qrow + 256, g0:g0 + 512].rearrange("(h d) t -> d h t", d=64), ["FT"], [kq])
                self.dma(q3[64:66], Cd["qaug"][2][:, :, g0:g0 + 512], (), [kq])
                for j in range(4):
                    mb = j % 2
                    c0 = g0 + j * 128
                    self.dma(mcst[mb], Cd["mconst"][c0:c0 + 128].rearrange("p a j -> p (a j)"), (), ["mc%d" % mb])
                    m3 = mcst[mb].rearrange("p (a j) -> p a j", a=3)
                    for h in range(4):
                        self.mm(self.bank(6)[:, h * 16:(h + 1) * 16], q3[0:64, h, j * 128:(j + 1) * 128], kmT[:, h, :], True, True, [kq, "kmT"], ["ps6"])
                    gs3 = gsm.rearrange("p (h j) -> p h j", h=4)
                    self.tt(gs3, self.bank(6)[:, 0:64].rearrange("p (h j) -> p h j", h=4), m3[:, 0, :].unsqueeze(1).to_broadcast([128, 4, 16]),
                            ALU.min, ["ps6", "mc%d" % mb], ["gsm"])
                    for h in range(4):
                        self.vmax(t8[:, h * 8:(h + 1) * 8], gsm[:, h * 16:(h + 1) * 16], ["gsm"], ["t8"])
                    s3 = selb.rearrange("p (h j) -> p h j", h=4)
                    for h in range(4):
                        self.ts(s3[:, h, :], gs3[:, h, :], t8[:, h * 8 + 2:h * 8 + 3], None, ALU.is_ge, None, ["gsm", "t8"], ["selb"])
                    self.tt(s3, s3, m3[:, 1, :].unsqueeze(1).to_broadcast([128, 4, 16]), ALU.mult, ["selb", "mc%d" % mb], ["selb"])
                    self.tt(s3, s3, m3[:, 2, :].unsqueeze(1).to_broadcast([128, 4, 16]), ALU.add, ["selb", "mc%d" % mb], ["selb"])
                    self.ts(selb, selb, -NEG, NEG, ALU.mult, ALU.add, ["selb"], ["selb"])
                    self.tr(self.bank(7)[0:64, 0:128], selb, ["selb", "ident"], ["ps7"])
                    self.cp(selbT[:, j * 128:(j + 1) * 128], self.bank(7)[0:64, 0:128], ["ps7"], ["selbT"])
            else:
                self.dma(q3[0:96], FT[qrow:qrow + 512, g0:g0 + 512].rearrange("(h r) t -> r h t", r=128)[0:96], ["FT"], [kq])
            nk = 4 * g + 4
            for h in range(4):
                ab = 2 + h % 2
                tb = 4 + h % 2
                for n_ in range(nk + 1):
                    if n_ < nk:
                        kt = n_
                        bk = cnt % 2
                        pt = cnt % 3
                        pk = "ps%d" % bk
                        diag = kt >= 4 * g
                        self.mm(self.bank(bk), KT[0:KR, h, kt * 128:(kt + 1) * 128], q3[0:KR, h, :], True, (not moba) and (not diag), ["KT", kq], [pk])
                        if moba:
                            self.mm(self.bank(bk), Embf[:, h * 16 + kt // 2, :], selbT, False, not diag, ["Embf", "selbT"], [pk])
                        if diag:
                            self.mm(self.bank(bk), identb, t512[:, kt - 4 * g, :], False, True, ["identb", "t512"], [pk])
                        self.act(PT[pt], self.bank(bk), AF.Exp, [pk], ["PT%d" % pt])
                        ptl.append(pt)
                        cnt += 1
                    if n_ >= 1:
                        m_ = n_ - 1
                        pm = ptl[len(ptl) - 1 - (1 if n_ < nk else 0)]
                        self.mm(self.bank(ab)[0:65, :], V4[:, m_, h, :], PT[pm], m_ == 0, m_ == nk - 1, ["PT%d" % pm, "V"], ["ps%d" % ab])
                so = otsb[h % 2]
                ko = "otsb%d" % (h % 2)
                self.cp(so[0:65, :], self.bank(ab)[0:65, :], ["ps%d" % ab], [ko], eng=("scalar" if h % 2 == 0 else "vector"))
                for j in range(4):
                    self.tr(self.bank(tb)[:, j * 65:(j + 1) * 65], so[0:65, j * 128:(j + 1) * 128], [ko, "ident"], ["ps%d" % tb])
                for j in range(4):
                    self.cp(stat[:, j:j + 1], self.bank(tb)[:, j * 65 + 64:j * 65 + 65], ["ps%d" % tb], ["st"])
                self.rcp(stat[:, 0:4], stat[:, 0:4], ["st"], ["st"])
                for j in range(4):
                    self.ts(oc[:, j, h, :], self.bank(tb)[:, j * 65:j * 65 + 64], stat[:, j:j + 1], None, ALU.mult, None, ["ps%d" % tb, "st"], ["oc"])
            for j in range(4):
                c0 = g0 + j * 128
                self.dma(self.Od[c0:c0 + 128, ocol:ocol + 256], oc[:, j].rearrange("p h d -> p (h d)"), ["oc"], ["Od"], q="gpsimd")

    def ph_merge(self, l, xin):
        self.reset()
        wbr = self.A(8192).rearrange("p (n k d) -> p n k d", n=4, k=2)
        wout = self.A(8192).rearrange("p (k d) -> p k d", k=8)
        Gt = [self.A(4096), self.A(4096)]
        Ot = [self.A(1024), self.A(1024)]
        OT = self.A(1024).rearrange("p (k t) -> p k t", k=8)
        mp = self.A(1024)
        mpT = self.A(1024).rearrange("p (k t) -> p k t", k=8)
        xt = [self.A(1024), self.A(1024)]
        tmp = self.A(512)
        xo = [self.A(1024), self.A(1024)]
        self.dma(wbr, self.Wl("w_branch", l).rearrange("n (k p) d -> p n k d", p=128), (), ["wbr"])
        self.dma(wout, self.Wl("w_out", l).rearrange("(k p) d -> p k d", p=128), (), ["wout"])
        cnt = 0
        for i in range(NT):
            b = i % 2
            c0 = i * 128
            self.dma(Ot[b], self.Od[c0:c0 + 128, :], ["Od"], ["Ot%d" % b])
            self.dma(Gt[b], self.Pall[c0:c0 + 128, NQKV:DIN], ["Pall"], ["Gt%d" % b])
            self.dma(xt[b], xin[c0:c0 + 128, :], (), ["xt%d" % b])
            self.act(Gt[b], Gt[b], AF.Sigmoid, ["Gt%d" % b], ["Gt%d" % b])
            self.transpose8(Ot[b], OT, 0, "Ot%d" % b, "OT", i)
            for cc in range(2):
                for n in range(4):
                    bk = cnt % 4
                    cnt += 1
                    pk = "ps%d" % bk
                    for k in range(2):
                        self.mm(self.bank(bk), OT[:, 2 * n + k, :], wbr[:, n, k, cc * 512:(cc + 1) * 512], k == 0, k == 1, ["OT", "wbr"], [pk])
                    gsl = Gt[b][:, n * 1024 + cc * 512:n * 1024 + (cc + 1) * 512]
                    if n == 0:
                        self.tt(mp[:, cc * 512:(cc + 1) * 512], self.bank(bk), gsl, ALU.mult, [pk, "Gt%d" % b], ["mp"])
                    else:
                        self.tt(tmp, self.bank(bk), gsl, ALU.mult, [pk, "Gt%d" % b], ["tmp"])
                        self.tt(mp[:, cc * 512:(cc + 1) * 512], mp[:, cc * 512:(cc + 1) * 512], tmp, ALU.add, ["mp", "tmp"], ["mp"])
            self.transpose8(mp, mpT, 0, "mp", "mpT", i + 1)
            for cc in range(2):
                bk = cnt % 4
                cnt += 1
                pk = "ps%d" % bk
                for k in range(8):
                    self.mm(self.bank(bk), mpT[:, k, :], wout[:, k, cc * 512:(cc + 1) * 512], k == 0, k == 7, ["mpT", "wout"], [pk])
                self.tt(tmp, self.bank(bk), self.mod[:, 2048 + cc * 512:2048 + (cc + 1) * 512], ALU.mult, [pk, "mod"], ["tmp"])
                self.tt(xo[b][:, cc * 512:(cc + 1) * 512], xt[b][:, cc * 512:(cc + 1) * 512], tmp, ALU.add, ["xt%d" % b, "tmp"], ["xo%d" % b])
            self.dma(self.xmid[c0:c0 + 128, :], xo[b], ["xo%d" % b], ["xmid"], q="gpsimd")

    def ph_moe(self, l, xout):
        self.reset()
        h2T = self.A(4096).rearrange("p (k t) -> p k t", k=8)
        acc = self.A(4096).rearrange("p (j d) -> p j d", j=4)
        xm = self.A(4096).rearrange("p (j d) -> p j d", j=4)
        ht = [self.A(1024), self.A(1024)]
        junk = self.A(1024)
        w13b = [self.A(4096), self.A(4096)]
        w2b = [self.A(2048), self.A(2048)]
        actT = [self.A(1024), self.A(1024)]
        sil = self.A(512)
        wr = self.A(8 * 36).rearrange("p (k n) -> p k n", k=8)
        brb = self.A(36)
        lg = self.A(36)
        Wr = self.A(128).rearrange("p (j e) -> p j e", j=4)
        stat = self.A(32)
        top8 = self.A(8)
        lfm = self.A(32)
        sel = self.A(32)
        we = self.A(32)
        oh = self.A(4)
        gm = self.A(32)
        xo = [self.A(1024), self.A(1024)]
        self.dma(wr[:, :, 0:4], self.Wl("w_coarse", l).rearrange("(k p) n -> p k n", p=128), (), ["wr"])
        self.dma(wr[:, :, 4:36], self.Wl("w_fine", l).rearrange("(k p) n -> p k n", p=128), (), ["wr"])
        self.dma(brb[:, 0:4], self._bc_row(self.Wd["b_coarse"][l:l + 1, :], 4), (), ["brb"])
        self.dma(brb[:, 4:36], self._bc_row(self.Wd["b_fine"][l:l + 1, :], 32), (), ["brb"])
        w13 = self.Wd["w13"][l]
        w2 = self.Wd["w2"][l]
        ecnt = 0
        ycnt = 0
        for g in range(8):
            for j in range(4):
                b = j % 2
                c0 = g * 512 + j * 128
                self.dma(xm[:, j, :], self.xmid[c0:c0 + 128, :], ["xmid"], ["xm%d" % j])
                self.norm_tile(xm[:, j, :], ht[b], junk, stat, j, 4096, 3072, "xm%d" % j, "ht%d" % b)
                self.transpose8(ht[b], h2T, j * 128, "ht%d" % b, "h2T", j)
                for k in range(8):
                    self.mm(self.bank(5)[:, 0:36], h2T[:, k, j * 128:(j + 1) * 128], wr[:, k, :], k == 0, k == 7, ["h2T", "wr"], ["ps5"])
                self.tt(lg, self.bank(5)[:, 0:36], brb, ALU.add, ["ps5", "brb"], ["lg"])
                self.red(stat[:, 8:9], lg[:, 0:4], ALU.max, ["lg"], ["rs"])
                self.ts(oh, lg[:, 0:4], stat[:, 8:9], None, ALU.is_ge, None, ["lg", "rs"], ["oh"])
                self.ts(stat[:, 9:10], stat[:, 8:9], -1.0, None, ALU.mult, None, ["rs"], ["rs"])
                self.act(junk[:, 0:4], lg[:, 0:4], AF.Exp, ["lg", "rs"], ["junk", "rs"], bias=stat[:, 9:10], accum=stat[:, 10:11])
                self.ts(gm.rearrange("p (g e) -> p g e", g=4), oh.unsqueeze(2).to_broadcast([128, 4, 8]), 1e9, -1e9, ALU.mult, ALU.add, ["oh"], ["gm"])
                self.tt(lfm, lg[:, 4:36], gm, ALU.add, ["lg", "gm"], ["lfm"])
                self.vmax(top8, lfm, ["lfm"], ["top8"])
                self.ts(sel, lfm, top8[:, 1:2], None, ALU.is_ge, None, ["lfm", "top8"], ["sel"])
                self.ts(stat[:, 11:12], top8[:, 0:1], -1.0, None, ALU.mult, None, ["top8"], ["rs"])
                self.act(we, lfm, AF.Exp, ["lfm", "rs"], ["we"], bias=stat[:, 11:12])
                self.tt(we, we, sel, ALU.mult, ["we", "sel"], ["we"])
                self.red(stat[:, 12:13], we, ALU.add, ["we"], ["rs"])
                self.tt(stat[:, 13:14], stat[:, 12:13], stat[:, 10:11], ALU.mult, ["rs"], ["rs"])
                self.rcp(stat[:, 13:14], stat[:, 13:14], ["rs"], ["rs"])
                self.ts(Wr[:, j, :], we, stat[:, 13:14], None, ALU.mult, None, ["we", "rs"], ["Wr"])
            for e in range(32):
                b = ecnt % 2
                ecnt += 1
                w13v = w13b[b].rearrange("p (k n) -> p k n", k=8)
                w2v = w2b[b].rearrange("p (k d) -> p k d", k=2)
                self.dma(w13v, w13[e].rearrange("(k p) n -> p k n", p=128), (), ["w13b%d" % b])
                self.dma(w2v, w2[e].rearrange("(k p) d -> p k d", p=128), (), ["w2b%d" % b])
                for m in range(4):
                    for k in range(8):
                        self.mm(self.bank(m), w13v[:, k, m * 128:(m + 1) * 128], h2T[:, k, :], k == 0, k == 7, ["w13b%d" % b, "h2T"], ["ps%d" % m])
                ab = actT[b]
                for kc in range(2):
                    self.act(sil, self.bank(kc), AF.Silu, ["ps%d" % kc], ["sil"])
                    self.tt(ab[:, kc * 512:(kc + 1) * 512], sil, self.bank(2 + kc), ALU.mult, ["sil", "ps%d" % (2 + kc)], ["actT%d" % b])
                for j in range(4):
                    for cc in range(2):
                        bk = 4 + ycnt % 4
                        ycnt += 1
                        pk = "ps%d" % bk
                        for kc in range(2):
                            self.mm(self.bank(bk), ab[:, kc * 512 + j * 128:kc * 512 + (j + 1) * 128], w2v[:, kc, cc * 512:(cc + 1) * 512], kc == 0, kc == 1,
                                    ["actT%d" % b, "w2b%d" % b], [pk])
                        a_sl = acc[:, j, cc * 512:(cc + 1) * 512]
                        if e == 0:
                            self.ts(a_sl, self.bank(bk), Wr[:, j, e:e + 1], None, ALU.mult, None, [pk, "Wr"], ["acc%d" % j])
                        else:
                            self.stt(a_sl, self.bank(bk), Wr[:, j, e:e + 1], a_sl, ALU.mult, ALU.add, [pk, "Wr", "acc%d" % j], ["acc%d" % j])
            for j in range(4):
                b = j % 2
                c0 = g * 512 + j * 128
                self.tt(xo[b], acc[:, j, :], self.mod[:, 5120:6144], ALU.mult, ["acc%d" % j, "mod"], ["xo%d" % b])
                self.tt(xo[b], xo[b], xm[:, j, :], ALU.add, ["xo%d" % b, "xm%d" % j], ["xo%d" % b])
                d = self.dma(xout[c0:c0 + 128, :], xo[b], ["xo%d" % b], ["xout"], q="gpsimd")
                self.outs.append(d)


    def idma(self, out, in_, out_off, in_off, r, w):
        oo = bass.IndirectOffsetOnAxis(ap=out_off, axis=0) if out_off is not None else None
        io = bass.IndirectOffsetOnAxis(ap=in_off, axis=0) if in_off is not None else None
        return self.S.add("gpsimd", lambda e: e.indirect_dma_start(out=out, out_offset=oo, in_=in_, in_offset=io), r, w, dma=True)

    def ph_moe_sparse(self, l, xout):
        self.reset()
        Cd = self.Cd
        selAll = self.A(1024).rearrange("p (g e) -> p g e", g=32)
        WrAll = self.A(1024).rearrange("p (g e) -> p g e", g=32)
        CAll = self.A(1024).rearrange("p (g e) -> p g e", g=32)
        idxf = self.A(64)
        idxAll = self.A(64).bitcast(I32).rearrange("p (g k) -> p g k", g=32)
        wAll = self.A(64).rearrange("p (g k) -> p g k", g=32)
        tot = self.A(32)
        U = self.A(128)
        wr = self.A(8 * 36).rearrange("p (k n) -> p k n", k=8)
        brb = self.A(36)
        bstart = self.A(NBLK)
        off13 = self.A(8)
        off2 = self.A(2)
        pstart = self.A(32)
        pend = self.A(32)
        padf = self.A(32)
        cnti = self.A(32).bitcast(I32)
        blke = self.A(NBLK)
        w13if = self.A(NBLK * 8)
        w13i = self.A(NBLK * 8).bitcast(I32).rearrange("p (b k) -> p b k", k=8)
        w2if = self.A(NBLK * 2)
        w2i = self.A(NBLK * 2).bitcast(I32).rearrange("p (b k) -> p b k", k=2)
        h2T = self.A(1024).rearrange("p (k t) -> p k t", k=8)
        xm = [self.A(1024), self.A(1024)]
        ht = [self.A(1024), self.A(1024)]
        junk = self.A(1024)
        lg = self.A(36)
        stat = self.A(32)
        top8 = self.A(8)
        lfm = self.A(32)
        we = self.A(32)
        oh = self.A(4)
        gm = self.A(32)
        Dt = self.A(32)
        mh = self.A(32)
        self.dma(wr[:, :, 0:4], self.Wl("w_coarse", l).rearrange("(k p) n -> p k n", p=128), (), ["wr"])
        self.dma(wr[:, :, 4:36], self.Wl("w_fine", l).rearrange("(k p) n -> p k n", p=128), (), ["wr"])
        self.dma(brb[:, 0:4], self._bc_row(self.Wd["b_coarse"][l:l + 1, :], 4), (), ["brb"])
        self.dma(brb[:, 4:36], self._bc_row(self.Wd["b_fine"][l:l + 1, :], 32), (), ["brb"])
        self.dma(U, Cd["U"], (), ["U"])
        self.dma(bstart, Cd["bstart"], (), ["bstart"])
        self.dma(off13, Cd["off13"], (), ["off"])
        self.dma(off2, Cd["off2"], (), ["off"])
        self.mset(tot, 0.0, ["tot"])
        for gi in range(NT):
            b = gi % 2
            c0 = gi * 128
            self.dma(xm[b], self.xmid[c0:c0 + 128, :], ["xmid"], ["xm%d" % b])
            self.norm_tile(xm[b], ht[b], junk, stat, b, 4096, 3072, "xm%d" % b, "ht%d" % b)
            self.dma(self.H2d[c0:c0 + 128, :], ht[b], ["ht%d" % b], ["H2d"], q="gpsimd")
            self.transpose8(ht[b], h2T, 0, "ht%d" % b, "h2T", gi)
            for k in range(8):
                self.mm(self.bank(5)[:, 0:36], h2T[:, k, :], wr[:, k, :], k == 0, k == 7, ["h2T", "wr"], ["ps5"])
            self.tt(lg, self.bank(5)[:, 0:36], brb, ALU.add, ["ps5", "brb"], ["lg"])
            self.red(stat[:, 8:9], lg[:, 0:4], ALU.max, ["lg"], ["rs"])
            self.ts(oh, lg[:, 0:4], stat[:, 8:9], None, ALU.is_ge, None, ["lg", "rs"], ["oh"])
            self.ts(stat[:, 9:10], stat[:, 8:9], -1.0, None, ALU.mult, None, ["rs"], ["rs"])
            self.act(junk[:, 0:4], lg[:, 0:4], AF.Exp, ["lg", "rs"], ["junk", "rs"], bias=stat[:, 9:10], accum=stat[:, 10:11])
            self.ts(gm.rearrange("p (g e) -> p g e", g=4), oh.unsqueeze(2).to_broadcast([128, 4, 8]), 1e9, -1e9, ALU.mult, ALU.add, ["oh"], ["gm"])
            self.tt(lfm, lg[:, 4:36], gm, ALU.add, ["lg", "gm"], ["lfm"])
            self.vmax(top8, lfm, ["lfm"], ["top8"])
            sel = selAll[:, gi, :]
            self.ts(sel, lfm, top8[:, 1:2], None, ALU.is_ge, None, ["lfm", "top8"], ["sel"])
            self.ts(stat[:, 11:12], top8[:, 0:1], -1.0, None, ALU.mult, None, ["top8"], ["rs"])
            self.act(we, lfm, AF.Exp, ["lfm", "rs"], ["we"], bias=stat[:, 11:12])
            self.tt(we, we, sel, ALU.mult, ["we", "sel"], ["we"])
            self.red(stat[:, 12:13], we, ALU.add, ["we"], ["rs"])
            self.tt(stat[:, 13:14], stat[:, 12:13], stat[:, 10:11], ALU.mult, ["rs"], ["rs"])
            self.rcp(stat[:, 13:14], stat[:, 13:14], ["rs"], ["rs"])
            self.ts(WrAll[:, gi, :], we, stat[:, 13:14], None, ALU.mult, None, ["we", "rs"], ["Wr"])
            self.mm(self.bank(4)[:, 0:32], U, sel, True, True, ["U", "sel"], ["ps4"])
            self.mm(self.bank(4)[:, 32:64], self.ones, sel, True, True, ["ones", "sel"], ["ps4"])
            self.tt(CAll[:, gi, :], self.bank(4)[:, 0:32], tot, ALU.add, ["ps4", "tot"], ["CAll"])
            self.tt(tot, self.bank(4)[:, 32:64], tot, ALU.add, ["ps4", "tot"], ["tot"])
        self.ts(cnti, tot, 127.0, None, ALU.add, None, ["tot"], ["cnti"])
        self.S.add("vector", lambda e: e.tensor_scalar(out=cnti, in0=cnti, scalar1=7, scalar2=7, op0=ALU.arith_shift_right, op1=ALU.logical_shift_left), ["cnti"], ["cnti"])
        self.cp(padf, cnti, ["cnti"], ["padf"])
        self.S.add("vector", lambda e: e.tensor_tensor_scan(out=pend, data0=self.ones[:, 0:32], data1=padf, initial=0.0, op0=ALU.mult, op1=ALU.add), ["ones", "padf"], ["pend"])
        self.tt(pstart, pend, padf, ALU.subtract, ["pend", "padf"], ["pstart"])
        cmp3 = self.A(NBLK * 32).rearrange("p (b e) -> p b e", e=32)
        self.tt(cmp3, pend.unsqueeze(1).to_broadcast([128, NBLK, 32]), bstart.unsqueeze(2).to_broadcast([128, NBLK, 32]), ALU.is_le, ["pend", "bstart"], ["cmp3"])
        self.red(blke, cmp3, ALU.add, ["cmp3"], ["blke"])
        self.ts(blke, blke, 31.0, None, ALU.min, None, ["blke"], ["blke"])
        self.ts(w13if[:, 0:NBLK], blke, 128.0, float(l * 4096), ALU.mult, ALU.add, ["blke"], ["w13if"])
        self.tt(w13if[:, 0:NBLK], w13if[:, 0:NBLK], off13[:, 0:1].to_broadcast([128, NBLK]), ALU.add, ["w13if", "off"], ["w13if"])
        widx = w13i.rearrange("p b k -> p (b k)")[:, 0:NBLK]
        self.cp(widx, w13if[:, 0:NBLK], ["w13if"], ["widx"])
        hb = [self.A(1024), self.A(1024)]
        xskeys = []
        for gi in range(NT):
            b = gi % 2
            c0 = gi * 128
            self.tt(Dt, CAll[:, gi, :], pstart, ALU.add, ["CAll", "pstart"], ["Dt"])
            self.stt(Dt, Dt, 1.0, selAll[:, gi, :], ALU.add, ALU.mult, ["Dt", "sel"], ["Dt"])
            self.vmax(top8, Dt, ["Dt"], ["top8"])
            for k in range(2):
                self.ts(mh, Dt, top8[:, k:k + 1], None, ALU.is_equal, None, ["Dt", "top8"], ["mh"])
                self.tt(mh, mh, WrAll[:, gi, :], ALU.mult, ["mh", "Wr"], ["mh"])
                self.red(wAll[:, gi, k:k + 1], mh, ALU.add, ["mh"], ["wAll"])
            self.ts(idxf[:, 2 * gi:2 * gi + 2], top8[:, 0:2], -1.0, None, ALU.add, None, ["top8"], ["idxf"])
            self.cp(idxAll[:, gi, :], idxf[:, 2 * gi:2 * gi + 2], ["idxf"], ["idxAll"])
            self.dma(hb[b], self.H2d[c0:c0 + 128, :], ["H2d"], ["hb%d" % b])
            for k in range(2):
                key = "Xs%d_%d" % (gi, k)
                xskeys.append(key)
                self.idma(self.Xsort[:, :], hb[b], idxAll[:, gi, k:k + 1], None, ["hb%d" % b, "idxAll"], [key])
        xb = [self.A(1024), self.A(1024)]
        xbT = self.A(1024).rearrange("p (k t) -> p k t", k=8)
        w13b = [self.A(4096).rearrange("p (k n) -> p k n", k=8) for _ in range(2)]
        w2b = [self.A(2048).rearrange("p (k d) -> p k d", k=2) for _ in range(2)]
        sil = self.A(256)
        actv = self.A(256)
        actT = self.A(256).rearrange("p (k t) -> p k t", k=2)
        ysb = [self.A(1024), self.A(1024)]
        w13flat = self.Wd["w13"].rearrange("l e (p k) n -> (l e p) (k n)", k=8)
        w2flat = self.Wd["w2"].rearrange("l e (p k) d -> (l e p) (k d)", k=2)
        yskeys = []
        cnt = 0
        for bi in range(NBLK):
            b = bi % 2
            self.dma(xb[b], self.Xsort[bi * 128:(bi + 1) * 128, :], xskeys, ["xb%d" % b])
            self.idma(w13b[b].rearrange("p k n -> p (k n)"), w13flat[:, :], None, widx[:, bi:bi + 1], ["widx"], ["w13b%d" % b])
            self.idma(w2b[b].rearrange("p k d -> p (k d)"), w2flat[:, :], None, widx[:, bi:bi + 1], ["widx"], ["w2b%d" % b])
            for j in range(2):
                tbk = 6 + j
                for kk in range(4):
                    k = 4 * j + kk
                    self.tr(self.bank(tbk)[:, kk * 128:(kk + 1) * 128], xb[b][:, k:1024:8], ["xb%d" % b, "ident"], ["ps%d" % tbk])
                self.cp(xbT[:, 4 * j:4 * j + 4, :], self.bank(tbk).rearrange("p (k t) -> p k t", k=4), ["ps%d" % tbk], ["xbT"],
                        eng=("scalar" if (j + bi) % 2 == 0 else "vector"))
            bk = cnt % 4
            cnt += 1
            for k in range(8):
                self.mm(self.bank(bk), xbT[:, k, :], w13b[b][:, k, :], k == 0, k == 7, ["xbT", "w13b%d" % b], ["ps%d" % bk])
            self.act(sil, self.bank(bk)[:, 0:256], AF.Silu, ["ps%d" % bk], ["sil"])
            self.tt(actv, sil, self.bank(bk)[:, 256:512], ALU.mult, ["sil", "ps%d" % bk], ["actv"])
            for kc in range(2):
                self.tr(self.bank(5)[:, kc * 128:(kc + 1) * 128], actv[:, kc:256:2], ["actv", "ident"], ["ps5"])
            self.cp(actT.rearrange("p k t -> p (k t)"), self.bank(5)[:, 0:256], ["ps5"], ["actT"], eng="scalar")
            for cc in range(2):
                bk = cnt % 4
                cnt += 1
                for kc in range(2):
                    self.mm(self.bank(bk), actT[:, kc, :], w2b[b][:, kc, cc * 512:(cc + 1) * 512], kc == 0, kc == 1, ["actT", "w2b%d" % b], ["ps%d" % bk])
                self.cp(ysb[b][:, cc * 512:(cc + 1) * 512], self.bank(bk), ["ps%d" % bk], ["ysb%d" % b], eng=("scalar" if cc == 0 else "vector"))
            key = "Ys%d" % bi
            yskeys.append(key)
            self.dma(self.Ysort[bi * 128:(bi + 1) * 128, :], ysb[b], ["ysb%d" % b], [key], q="sync")
        Y = [[self.A(1024), self.A(1024)] for _ in range(2)]
        xo = [self.A(1024), self.A(1024)]
        for gi in range(NT):
            b = gi % 2
            c0 = gi * 128
            self.dma(xm[b], self.xmid[c0:c0 + 128, :], ["xmid"], ["xm%d" % b])
            for k in range(2):
                self.idma(Y[b][k], self.Ysort[:, :], None, idxAll[:, gi, k:k + 1], yskeys + ["idxAll"], ["Y%d%d" % (b, k)])
            self.ts(xo[b], Y[b][0], wAll[:, gi, 0:1], None, ALU.mult, None, ["Y%d0" % b, "wAll"], ["xo%d" % b])
            self.stt(xo[b], Y[b][1], wAll[:, gi, 1:2], xo[b], ALU.mult, ALU.add, ["Y%d1" % b, "wAll", "xo%d" % b], ["xo%d" % b])
            self.tt(xo[b], xo[b], self.mod[:, 5120:6144], ALU.mult, ["xo%d" % b, "mod"], ["xo%d" % b])
            self.tt(xo[b], xo[b], xm[b], ALU.add, ["xo%d" % b, "xm%d" % b], ["xo%d" % b])
            d = self.dma(xout[c0:c0 + 128, :], xo[b], ["xo%d" % b], ["xout"], q="sync")
            self.outs.append(d)

_CONSTS = None


def _consts():
    global _CONSTS
    if _CONSTS is None:
        _CONSTS = make_consts()
    return _CONSTS


def _in_map(x_b, c_b, weights, layers):
    m = {"x": np.ascontiguousarray(x_b, dtype=np.float32), "c": np.ascontiguousarray(c_b.reshape(8, 128), dtype=np.float32)}
    for name, _ in WEIGHT_SPECS:
        m[name] = np.ascontiguousarray(weights[name][layers])
    for name, _, _ in CONST_SPECS:
        m["k_" + name] = _consts()[name]
    return m


FUSED = True


def kernel(**inputs):
    x = np.asarray(inputs["x"], dtype=np.float32)
    c = np.asarray(inputs["c"], dtype=np.float32)
    weights = {name: np.asarray(inputs[name], dtype=np.float32) for name, _ in WEIGHT_SPECS}
    n = 8
    if FUSED:
        nc = Builder(DEPTH).build()
        layers = list(range(DEPTH))
        in_maps = [_in_map(x[i % 4], c[i % 4], weights, layers) for i in range(n)]
        res = run_bass_kernel_spmd(nc, in_maps, core_ids=list(range(n)))
        return np.stack([res.results[i]["y"] for i in range(4)], axis=0).astype(np.float32)
    nc = Builder(1).build()
    cur = [x[i] for i in range(4)]
    for l in range(DEPTH):
        in_maps = [_in_map(cur[i % 4], c[i % 4], weights, [l]) for i in range(n)]
        res = run_bass_kernel_spmd(nc, in_maps, core_ids=list(range(n)))
        cur = [np.asarray(res.results[i]["y"]) for i in range(4)]
    return np.stack(cur, axis=0).astype(np.float32)
```

```python
import math
import numpy as np
import ml_dtypes
import concourse.bass as bass
import concourse.mybir as mybir
from concourse.bass_utils import run_bass_kernel_spmd
from contextlib import ExitStack

F32 = mybir.dt.float32
BF16 = mybir.dt.bfloat16
F32R = mybir.dt.float32r
AF = mybir.ActivationFunctionType
ALU = mybir.AluOpType
AX = mybir.AxisListType

SEQ = 4096
D = 1024
NT = 32
DIN = 6572
NQKV = 2476
DEPTH = 4
NEG = -30000.0
EPS = 1e-6
NF = 2688
SB_FLOATS = 52000
PERSIST = 8192
NBLK = 96
SPARSE_MOE = True
I32 = mybir.dt.int32


class Op:
    __slots__ = ("eng", "fn", "dma", "deps", "signal", "sigval", "dsem", "dval", "idx")

    def __init__(self, eng, fn, dma):
        self.eng = eng
        self.fn = fn
        self.dma = dma
        self.deps = None
        self.signal = False
        self.sigval = 0
        self.dsem = None
        self.dval = 0


class Sched:
    ENGS = ("tensor", "vector", "scalar", "gpsimd", "sync")

    def __init__(self, nc, n_dma_sems=48):
        self.nc = nc
        self.ops = {e: [] for e in self.ENGS}
        self.last_w = {}
        self.readers = {}
        self.n_dma_sems = n_dma_sems
        self.dma_uses = [0] * n_dma_sems
        self.dma_last = [None] * n_dma_sems
        self.dma_rr = 0

    def add(self, eng, fn, reads=(), writes=(), dma=False):
        op = Op(eng, fn, dma)
        deps = {}

        def adddep(d):
            if d is op:
                return
            if d.dma:
                deps[("d", id(d))] = d
            else:
                k = ("e", d.eng)
                o = deps.get(k)
                if o is None or o.idx < d.idx:
                    deps[k] = d

        for r in reads:
            w = self.last_w.get(r)
            if w is not None:
                adddep(w)
        for w_ in writes:
            w = self.last_w.get(w_)
            if w is not None:
                adddep(w)
            rd = self.readers.get(w_)
            if rd:
                for d in rd.values():
                    adddep(d)
        if dma:
            i = self.dma_rr
            self.dma_rr = (self.dma_rr + 1) % self.n_dma_sems
            prev = self.dma_last[i]
            if prev is not None:
                adddep(prev)
            self.dma_uses[i] += 1
            op.dsem = i
            op.dval = 16 * self.dma_uses[i]
            self.dma_last[i] = op
        op.idx = len(self.ops[eng])
        op.deps = list(deps.values())
        for d in op.deps:
            d.signal = True
        for r in reads:
            rd = self.readers.get(r)
            if rd is None:
                rd = self.readers[r] = {}
            rd[("d", id(op)) if dma else ("e", eng)] = op
        for w_ in writes:
            self.last_w[w_] = op
            self.readers[w_] = {}
        self.ops[eng].append(op)
        return op

    def barrier(self):
        lasts = []
        for e in self.ENGS:
            for o in reversed(self.ops[e]):
                if o.fn is not None and not o.dma:
                    lasts.append(o)
                    break
        dmas = [d for d in self.dma_last if d is not None]
        for e in self.ENGS:
            op = Op(e, None, False)
            op.idx = len(self.ops[e])
            op.deps = list(lasts) + dmas
            for d in op.deps:
                d.signal = True
            self.ops[e].append(op)
        self.last_w = {}
        self.readers = {}

    def emit(self, final_waits=()):
        nc = self.nc
        with ExitStack() as es:
            esem = {e: es.enter_context(nc.semaphore("s_" + e)) for e in self.ENGS}
            dsem = [es.enter_context(nc.semaphore("d%d" % i)) for i in range(self.n_dma_sems)]
            for d in final_waits:
                d.signal = True
            for e in self.ENGS:
                c = 0
                for op in self.ops[e]:
                    if op.signal and not op.dma and op.fn is not None:
                        c += 1
                        op.sigval = c
            block = es.enter_context(nc.Block())

            def run(e, eng):
                known = {}
                for op in self.ops[e]:
                    for d in op.deps:
                        if d.dma:
                            key, sem, val = ("d", d.dsem), dsem[d.dsem], d.dval
                        else:
                            if d.eng == e and e == "tensor":
                                continue
                            key, sem, val = ("e", d.eng), esem[d.eng], d.sigval
                        if known.get(key, 0) >= val:
                            continue
                        eng.wait_ge(sem, val)
                        known[key] = val
                    if op.fn is None:
                        continue
                    inst = op.fn(eng)
                    if op.dma:
                        inst.then_inc(dsem[op.dsem], 16)
                    elif op.signal:
                        inst.then_inc(esem[e], 1)
                if e == "sync":
                    for d in final_waits:
                        if d.dma:
                            eng.wait_ge(dsem[d.dsem], d.dval)
                        else:
                            eng.wait_ge(esem[d.eng], d.sigval)

            @block.tensor
            def _(eng):
                run("tensor", eng)

            @block.vector
            def _(eng):
                run("vector", eng)

            @block.scalar
            def _(eng):
                run("scalar", eng)

            @block.gpsimd
            def _(eng):
                run("gpsimd", eng)

            @block.sync
            def _(eng):
                run("sync", eng)


def alibi_slopes():
    n = 12

    def pow2(m):
        start = 2.0 ** (-8.0 / m)
        return [start ** (i + 1) for i in range(m)]

    c = 2 ** int(math.floor(math.log2(n)))
    s = pow2(c) + (pow2(2 * c)[0::2][: n - c] if c < n else [])
    s = -np.sort(-np.asarray(s, np.float32))
    return s.reshape(4, 3).T


def make_consts():
    bf = ml_dtypes.bfloat16
    C = {}
    C["ident"] = np.eye(128, dtype=np.float32)
    C["identb"] = np.eye(128).astype(bf)
    sl = alibi_slopes().astype(np.float64)
    t = np.arange(SEQ, dtype=np.float64)
    qaug = np.zeros((3, 2, 4, SEQ), np.float32)
    for m in range(3):
        for h in range(4):
            qaug[m, 0, h] = (-sl[m, h] * t).astype(np.float32)
            qaug[m, 1, h] = np.float32(sl[m, h])
    C["qaug"] = qaug
    kaug = np.zeros((2, SEQ), np.float32)
    kaug[0] = 1.0
    kaug[1] = t
    C["kaug"] = kaug
    kaugc = np.zeros((2, 256), np.float32)
    kaugc[0] = 1.0
    kaugc[1] = 16.0 * np.arange(256) + 31.0
    C["kaugc"] = kaugc
    E = np.zeros((64, SEQ), np.float32)
    for j in range(64):
        E[j, j * 64:(j + 1) * 64] = 1.0
    C["E"] = E.astype(bf)
    Em = np.zeros((64, 64, 128), np.float32)
    for v in range(64):
        Em[v, v, :] = 1.0
    C["Em"] = Em.astype(bf)
    cc = np.arange(128)[:, None]
    rr = np.arange(128)[None, :]
    triA = np.where(cc > rr, NEG, 0.0).astype(np.float32)
    triB = np.where(cc <= rr, NEG, 0.0).astype(np.float32)
    C["triA"] = np.tile(triA, (1, 4)).astype(bf)
    C["triB"] = np.tile(triB, (1, 4)).astype(bf)
    t512 = np.zeros((128, 4, 512), np.float32)
    ql = np.arange(512)[None, :]
    for j in range(4):
        t512[:, j, :] = np.where(128 * j + cc > ql, NEG, 0.0)
    C["t512"] = t512.astype(bf)
    mcmp = np.zeros((128, 2, SEQ), np.float32)
    for nt in range(2):
        n = nt * 128 + np.arange(128)[:, None]
        bad = (n >= 255) | (16 * n + 31 > np.arange(SEQ)[None, :])
        mcmp[:, nt, :] = np.where(bad, NEG, 0.0)
    C["mcmp"] = mcmp.astype(bf)
    cur = (np.arange(SEQ) // 64)[:, None]
    j = np.arange(64)[None, :]
    forced = (j == 0) | (j == cur) | (j == cur - 1)
    valid = j <= cur
    selc = np.zeros((SEQ, 3, 64), np.float32)
    selc[:, 0, :] = np.where(forced, 1e4, 0.0)
    selc[:, 1, :] = np.where(valid, 1e30, -1e30)
    selc[:, 2, :] = np.where(valid, 0.0, NEG)
    C["selc"] = selc
    curb = (np.arange(SEQ) // 256)[:, None]
    jb = np.arange(16)[None, :]
    mc = np.zeros((SEQ, 3, 16), np.float32)
    mc[:, 0, :] = np.where(jb < curb, 1e30, -1e30)
    mc[:, 1, :] = np.where(jb < curb, 1.0, 0.0)
    mc[:, 2, :] = np.where(jb == curb, 1.0, 0.0)
    C["mconst"] = mc
    half = 16
    inv = 10000.0 ** (-np.arange(half, dtype=np.float32) / half)
    ang = np.arange(SEQ, dtype=np.float32)[:, None] * inv[None, :]
    C["rope"] = np.concatenate([np.cos(ang), np.sin(ang)], axis=1).astype(np.float32)
    n_cmp = 255
    starts = np.arange(n_cmp) * 16
    jb64 = np.arange(64) * 64
    cover = ((starts[:, None] < jb64[None, :] + 64) & (starts[:, None] + 32 > jb64[None, :])).astype(np.float32)
    cov = np.zeros((256, 64), np.float32)
    cov[:255] = cover
    C["cover"] = np.ascontiguousarray(cov.reshape(2, 128, 64).transpose(1, 0, 2))
    tp = np.arange(128)
    C["U"] = (tp[:, None] < tp[None, :]).astype(np.float32)
    C["bstart"] = np.tile((128.0 * np.arange(NBLK, dtype=np.float32))[None, :], (128, 1)).astype(np.float32)
    C["off13"] = (np.arange(8)[None, :] * 128 + tp[:, None]).astype(np.float32)
    C["off2"] = (np.arange(2)[None, :] * 128 + tp[:, None]).astype(np.float32)
    return C


CONST_SPECS = [
    ("ident", [128, 128], F32), ("identb", [128, 128], BF16), ("qaug", [3, 2, 4, SEQ], F32),
    ("kaug", [2, SEQ], F32), ("kaugc", [2, 256], F32), ("E", [64, SEQ], BF16),
    ("Em", [64, 64, 128], BF16), ("triA", [128, 512], BF16), ("triB", [128, 512], BF16),
    ("t512", [128, 4, 512], BF16), ("mcmp", [128, 2, SEQ], BF16), ("selc", [SEQ, 3, 64], F32),
    ("mconst", [SEQ, 3, 16], F32), ("rope", [SEQ, 32], F32), ("cover", [128, 2, 64], F32),
    ("U", [128, 128], F32), ("bstart", [128, NBLK], F32), ("off13", [128, 8], F32), ("off2", [128, 2], F32),
]

WEIGHT_SPECS = [
    ("w_ada", [D, 6 * D]), ("b_ada", [6 * D]), ("norm_gain", [2, D]), ("w_in", [D, DIN]),
    ("qk_gain", [10, 64]), ("cmp_pe", [2, 32, 64]), ("cmp_w1", [2, 2048, 256]), ("cmp_w2", [2, 256, 64]),
    ("swa_sinks", [4]), ("lat_gain_q", [384]), ("lat_gain_kv", [128]), ("rope_gain", [2, 32]),
    ("w_uq", [384, 384]), ("w_ukv", [128, 512]), ("w_branch", [4, 256, D]), ("w_out", [D, D]),
    ("w_coarse", [D, 4]), ("b_coarse", [4]), ("w_fine", [D, 32]), ("b_fine", [32]),
    ("w13", [32, D, 512]), ("w2", [32, 256, D]),
]


class Builder:
    def __init__(self, n_layers, debug=False, stop_after=None, r32=False):
        self.r32 = r32
        self.nl = n_layers
        self.debug = debug
        self.stop_after = stop_after
        nc = self.nc = bass.Bass("TRN2", target_bir_lowering=False)
        self.S = Sched(nc)
        self.x_in = nc.dram_tensor("x", [SEQ, D], F32, kind="ExternalInput").ap()
        self.c_in = nc.dram_tensor("c", [8, 128], F32, kind="ExternalInput").ap()
        self.Wd = {}
        for name, shp in WEIGHT_SPECS:
            self.Wd[name] = nc.dram_tensor(name, [n_layers] + shp, F32, kind="ExternalInput").ap()
        self.Cd = {}
        for name, shp, dt in CONST_SPECS:
            self.Cd[name] = nc.dram_tensor("k_" + name, shp, dt, kind="ExternalInput").ap()
        self.y = nc.dram_tensor("y", [SEQ, D], F32, kind="ExternalOutput").ap()
        dk = "ExternalOutput" if debug else "Internal"
        self.Pall = nc.dram_tensor("Pall", [SEQ, DIN], F32, kind=dk).ap()
        self.FT = nc.dram_tensor("FT", [NF, SEQ], F32, kind=dk).ap()
        self.Vd = nc.dram_tensor("Vd", [SEQ, 256], F32, kind=dk).ap()
        self.Od = nc.dram_tensor("Od", [SEQ, D], F32, kind=dk).ap()
        self.xmid = nc.dram_tensor("xmid", [SEQ, D], F32, kind=dk).ap()
        self.xpp = [nc.dram_tensor("xpp%d" % i, [SEQ, D], F32, kind="Internal").ap() for i in range(2)]
        self.H2d = nc.dram_tensor("H2d", [SEQ, D], F32, kind="Internal").ap()
        self.Xsort = nc.dram_tensor("Xsort", [NBLK * 128, D], F32, kind="Internal").ap()
        self.Ysort = nc.dram_tensor("Ysort", [NBLK * 128, D], F32, kind="Internal").ap()
        self.outs = []
        if debug:
            self.dbg1 = nc.dram_tensor("dbg1", [SEQ, 256], F32, kind="ExternalOutput").ap()
            self.dbg2 = nc.dram_tensor("dbg2", [SEQ, 128], F32, kind="ExternalOutput").ap()
            self.dbg3 = nc.dram_tensor("dbg3", [128, 2, 64], F32, kind="ExternalOutput").ap()
            self.dbg4 = nc.dram_tensor("dbg4", [128, 2, 129], F32, kind="ExternalOutput").ap()
            self.dbg5 = nc.dram_tensor("dbg5", [66, 256], F32, kind="ExternalOutput").ap()

    def dma(self, out, in_, r=(), w=(), q="sync"):
        return self.S.add(q, lambda e: e.dma_start(out=out, in_=in_), r, w, dma=True)

    def mm(self, out, lhsT, rhs, st, sp, r, w, fast=False):
        if fast and self.r32 and lhsT.dtype == F32 and rhs.dtype == F32:
            lhsT = lhsT.bitcast(F32R)
            rhs = rhs.bitcast(F32R)
        return self.S.add("tensor", lambda e: e.matmul(out, lhsT=lhsT, rhs=rhs, start=st, stop=sp), r, w)

    def tr(self, out, in_, r, w):
        p = in_.shape[0]
        idn = self.ident[0:p, 0:p]
        return self.S.add("tensor", lambda e: e.transpose(out=out, in_=in_, identity=idn), list(r), w)

    def act(self, out, in_, func, r, w, bias=None, scale=None, accum=None):
        kw = {}
        if bias is not None:
            kw["bias"] = bias
        if scale is not None:
            kw["scale"] = scale
        if accum is not None:
            kw["accum_out"] = accum
        return self.S.add("scalar", lambda e: e.activation(out=out, in_=in_, func=func, **kw), r, w)

    def tt(self, out, a, b, op, r, w, eng="vector"):
        return self.S.add(eng, lambda e: e.tensor_tensor(out=out, in0=a, in1=b, op=op), r, w)

    def ts(self, out, a, s1, s2, op0, op1, r, w, eng="vector"):
        if s2 is None:
            return self.S.add(eng, lambda e: e.tensor_scalar(out=out, in0=a, scalar1=s1, scalar2=None, op0=op0), r, w)
        return self.S.add(eng, lambda e: e.tensor_scalar(out=out, in0=a, scalar1=s1, scalar2=s2, op0=op0, op1=op1), r, w)

    def stt(self, out, a, s, b, op0, op1, r, w):
        return self.S.add("vector", lambda e: e.scalar_tensor_tensor(out=out, in0=a, scalar=s, in1=b, op0=op0, op1=op1), r, w)

    def red(self, out, in_, op, r, w):
        return self.S.add("vector", lambda e: e.tensor_reduce(out=out, in_=in_, axis=AX.X, op=op), r, w)

    def cp(self, out, in_, r, w, eng="vector"):
        if eng == "scalar":
            return self.S.add("scalar", lambda e: e.activation(out=out, in_=in_, func=AF.Copy), r, w)
        return self.S.add(eng, lambda e: e.tensor_copy(out=out, in_=in_), r, w)

    def rcp(self, out, in_, r, w):
        return self.S.add("vector", lambda e: e.reciprocal(out=out, in_=in_), r, w)

    def mset(self, ap, val, w):
        return self.S.add("vector", lambda e: e.memset(ap, val), (), w)

    def vmax(self, out, in_, r, w):
        return self.S.add("vector", lambda e: e.max(out=out, in_=in_), r, w)

    def rsq(self, ap, inv_n, key):
        self.ts(ap, ap, inv_n, EPS, ALU.mult, ALU.add, [key], [key])
        self.act(ap, ap, AF.Sqrt, [key], [key])
        self.rcp(ap, ap, [key], [key])

    def reset(self):
        self.S.barrier()
        self.top = PERSIST

    def A(self, n, parts=128):
        n = (n + 1) // 2 * 2
        ap = self.big[0:parts, self.top:self.top + n]
        self.top += n
        assert self.top <= SB_FLOATS, self.top
        return ap

    def bank(self, i):
        return self.ps[:, i * 512:(i + 1) * 512]

    def Wl(self, name, l):
        return self.Wd[name][l]

    def build(self):
        nc = self.nc
        with ExitStack() as es:
            self.big = es.enter_context(nc.sbuf_tensor("big", [128, SB_FLOATS], F32))
            self.ps = es.enter_context(nc.psum_tensor("ps", [128, 4096], F32))
            big = self.big
            self.mod = big[:, 0:6144]
            self.ident = big[:, 6144:6272]
            self.identb = big[:, 6272:6336].bitcast(BF16)
            self.ones = big[:, 6336:6464]
            self.gq = big[:, 6464:7104]
            self.esink = big[:, 7104:7108]
            self.top = PERSIST
            self.dma(self.ident, self.Cd["ident"], (), ["ident"])
            self.dma(self.identb, self.Cd["identb"], (), ["identb"])
            self.mset(self.ones, 1.0, ["ones"])
            for l in range(self.nl):
                xin = self.x_in if l == 0 else self.xpp[(l - 1) % 2]
                xout = self.y if l == self.nl - 1 else self.xpp[l % 2]
                self.layer(l, xin, xout)
            self.S.barrier()
            self.S.emit(final_waits=self.outs)
        return nc

    def layer(self, l, xin, xout):
        st = self.stop_after
        self.ph_mod(l)
        self.ph_inproj(l, xin)
        if st == "inproj":
            return
        self.ph_prep(l)
        if st == "prep":
            return
        self.ph_nsa(l)
        if st == "nsa":
            return
        self.ph_swa(l)
        if st == "swa":
            return
        self.ph_dense(l, moba=True)
        if st == "moba":
            return
        self.ph_dense(l, moba=False)
        if st == "mla":
            return
        self.ph_merge(l, xin)
        if st == "merge":
            return
        if SPARSE_MOE:
            self.ph_moe_sparse(l, xout)
        else:
            self.ph_moe(l, xout)

    def ph_mod(self, l):
        self.reset()
        c8 = self.A(128, 8)
        cT = self.A(8)
        rep = self.A(1024)
        bada = self.A(6144)
        ng = self.A(2048)
        wch = [self.A(4096), self.A(4096)]
        self.dma(c8, self.c_in, (), ["c8"])
        self.tr(self.bank(7)[:, 0:8], c8, ["c8", "ident"], ["ps7"])
        self.act(cT, self.bank(7)[:, 0:8], AF.Silu, ["ps7"], ["cT"])
        for k in range(8):
            self.ts(rep[:, k * 128:(k + 1) * 128], self.ones, cT[:, k:k + 1], None, ALU.mult, None, ["ones", "cT"], ["rep"])
        self.dma(bada, self._bc_row(self.Wd["b_ada"][l:l + 1, :], 6144), (), ["bada"])
        self.dma(ng, self._bc_row(self.Wd["norm_gain"][l:l + 1].rearrange("o a d -> o (a d)"), 2048), (), ["ng"])
        self.dma(self.gq, self._bc_row(self.Wd["qk_gain"][l:l + 1].rearrange("o a d -> o (a d)"), 640), (), ["gq"])
        wa = self.Wl("w_ada", l).rearrange("(kc p) n -> p kc n", p=128)
        for n in range(12):
            b = n % 2
            wv = wch[b].rearrange("p (k n) -> p k n", k=8)
            self.dma(wv, wa[:, :, n * 512:(n + 1) * 512], (), ["wch%d" % b])
            for k in range(8):
                self.mm(self.bank(b), rep[:, k * 128:(k + 1) * 128], wv[:, k, :], k == 0, k == 7, ["rep", "wch%d" % b], ["ps%d" % b])
            self.tt(self.mod[:, n * 512:(n + 1) * 512], self.bank(b), bada[:, n * 512:(n + 1) * 512], ALU.add, ["ps%d" % b, "bada"], ["mod"])
        self.stt(self.mod[:, 1024:2048], self.mod[:, 1024:2048], 1.0, ng[:, 0:1024], ALU.add, ALU.mult, ["mod", "ng"], ["mod"])
        self.stt(self.mod[:, 4096:5120], self.mod[:, 4096:5120], 1.0, ng[:, 1024:2048], ALU.add, ALU.mult, ["mod", "ng"], ["mod"])

    def _bc_row(self, row_ap, n):
        return row_ap.partition_broadcast(128).rearrange("p o n -> p (o n)") if len(row_ap.shape) == 2 else row_ap

    def norm_tile(self, xt, ht, junk, stat, col, a_off, s_off, kx, kh):
        sc = stat[:, col:col + 1]
        self.act(junk, xt, AF.Square, [kx], ["junk", "stat"], accum=sc)
        self.rsq(sc, 1.0 / D, "stat")
        self.stt(ht, xt, sc, self.mod[:, a_off:a_off + D], ALU.mult, ALU.mult, [kx, "stat", "mod"], [kh])
        self.tt(ht, ht, self.mod[:, s_off:s_off + D], ALU.add, [kh, "mod"], [kh])

    def transpose8(self, src, dst3, c0, ksrc, kdst, flip):
        for j in range(2):
            bk = 6 + j
            for kk in range(4):
                self.tr(self.bank(bk)[:, kk * 128:(kk + 1) * 128], src[:, (4 * j + kk) * 128:(4 * j + kk + 1) * 128], [ksrc, "ident"], ["ps%d" % bk])
            self.cp(dst3[:, 4 * j:4 * j + 4, c0:c0 + 128], self.bank(bk).rearrange("p (k t) -> p k t", k=4), ["ps%d" % bk], [kdst],
                    eng=("scalar" if (j + flip) % 2 == 0 else "vector"))

    def ph_inproj(self, l, xin):
        win = self.Wl("w_in", l).rearrange("(kc p) n -> p kc n", p=128)
        for hf in range(2):
            self.reset()
            hT = self.A(16384).rearrange("p (k t) -> p k t", k=8)
            xt = [self.A(1024), self.A(1024)]
            ht = [self.A(1024), self.A(1024)]
            junk = self.A(1024)
            stat = self.A(32)
            wch = [self.A(4096), self.A(4096)]
            stg = [self.A(512) for _ in range(3)]
            for ti in range(16):
                gi = hf * 16 + ti
                b = ti % 2
                self.dma(xt[b], xin[gi * 128:(gi + 1) * 128, :], (), ["xt%d" % b])
                self.norm_tile(xt[b], ht[b], junk, stat, ti, 1024, 0, "xt%d" % b, "ht%d" % b)
                self.transpose8(ht[b], hT, ti * 128, "ht%d" % b, "hT", ti)
            cnt = 0
            for cc in range(13):
                ncol = min(512, DIN - cc * 512)
                b = cc % 2
                wv = wch[b].rearrange("p (k n) -> p k n", k=8)[:, :, 0:ncol]
                self.dma(wv, win[:, :, cc * 512:cc * 512 + ncol], (), ["wch%d" % b])
                for ti in range(16):
                    gi = hf * 16 + ti
                    bk = cnt % 4
                    sg = cnt % 3
                    for k in range(8):
                        self.mm(self.bank(bk)[:, 0:ncol], hT[:, k, ti * 128:(ti + 1) * 128], wv[:, k, :], k == 0, k == 7, ["hT", "wch%d" % b], ["ps%d" % bk], fast=True)
                    self.cp(stg[sg][:, 0:ncol], self.bank(bk)[:, 0:ncol], ["ps%d" % bk], ["stg%d" % sg], eng=("scalar" if cnt % 2 == 0 else "vector"))
                    self.dma(self.Pall[gi * 128:(gi + 1) * 128, cc * 512:cc * 512 + ncol], stg[sg][:, 0:ncol], ["stg%d" % sg], ["Pall"], q="gpsimd")
                    cnt += 1

    def ph_prep(self, l):
        self.reset()
        gq = self.gq
        gainA = self.A(640)
        gainB = self.A(1024)
        lgq = self.A(384)
        lgkv = self.A(128)
        rg = self.A(64)
        gqd = self.A(192)
        wuq = self.A(3 * 384).rearrange("p (k n) -> p k n", k=3)
        wukv = self.A(512)
        Pt = [self.A(NQKV), self.A(NQKV)]
        cs = [self.A(32), self.A(32)]
        sq = self.A(1676)
        ZA = self.A(640)
        ZB = self.A(1024)
        cqn = self.A(512)
        cT = self.A(512).rearrange("p (k t) -> p k t", k=4)
        qf = self.A(512).rearrange("p (h d) -> p h d", h=4)
        kf = self.A(512).rearrange("p (h d) -> p h d", h=4)
        sqq = self.A(384)
        sqk = self.A(256)
        qr = self.A(128)
        krn = self.A(32)
        krot = self.A(32)
        tmp = [self.A(64) for _ in range(4)]
        vd = self.A(256)
        stat = self.A(64)
        trs = [self.A(512) for _ in range(3)]
        junk = self.A(384)
        self.dma(lgq, self._bc_row(self.Wd["lat_gain_q"][l:l + 1, :], 384), (), ["lg"])
        self.dma(lgkv, self._bc_row(self.Wd["lat_gain_kv"][l:l + 1, :], 128), (), ["lg"])
        self.dma(rg, self._bc_row(self.Wd["rope_gain"][l:l + 1].rearrange("o a d -> o (a d)"), 64), (), ["lg"])
        self.dma(wuq, self.Wl("w_uq", l).rearrange("(k p) n -> p k n", p=128), (), ["wu"])
        self.dma(wukv, self.Wl("w_ukv", l), (), ["wu"])
        self.mset(gainA, 1.0, ["gain"])
        self.mset(gainB, 1.0, ["gain"])

        def gset(dst, g0, ng_, slot, scale):
            o = dst[:, g0 * 64:(g0 + ng_) * 64].rearrange("p (g d) -> p g d", d=64)
            i = gq[:, slot * 64:(slot + 1) * 64].unsqueeze(1).to_broadcast([128, ng_, 64])
            self.ts(o, i, scale, None, ALU.mult, None, ["gq", "gain"], ["gain"])

        gset(gainA, 0, 4, 0, 0.125)
        gset(gainA, 6, 1, 2, 1.0)
        gset(gainA, 8, 1, 3, 1.0)
        gset(gainB, 0, 4, 4, 0.125)
        gset(gainB, 4, 2, 5, 1.0)
        gset(gainB, 8, 4, 6, 0.125)
        gset(gainB, 12, 4, 7, 1.0)
        sd = 96.0 ** -0.5
        self.ts(gqd[:, 0:64], gq[:, 512:576], sd, None, ALU.mult, None, ["gq", "lg"], ["gqd"])
        self.ts(gqd[:, 64:96], rg[:, 0:32], sd, None, ALU.mult, None, ["gq", "lg"], ["gqd"])
        self.ts(gqd[:, 96:160], gq[:, 576:640], 1.0, None, ALU.mult, None, ["gq", "lg"], ["gqd"])
        self.ts(gqd[:, 160:192], rg[:, 32:64], 1.0, None, ALU.mult, None, ["gq", "lg"], ["gqd"])
        self.mset(qf.rearrange("p h d -> p (h d)"), 0.0, ["qf"])
        self.mset(kf.rearrange("p h d -> p (h d)"), 0.0, ["kf"])
        tcnt = [0]

        def tgroup(blocks, row0, c0):
            g = tcnt[0]
            tcnt[0] += 1
            bk = g % 4
            sg = g % 3
            for i, (ap, key) in enumerate(blocks):
                self.tr(self.bank(bk)[:, i * 128:(i + 1) * 128], ap, [key, "ident"], ["ps%d" % bk])
            self.cp(trs[sg], self.bank(bk), ["ps%d" % bk], ["trs%d" % sg], eng=("scalar" if g % 2 == 0 else "vector"))
            self.dma(self.FT[row0:row0 + 512, c0:c0 + 128].rearrange("(b p) t -> p b t", p=128),
                     trs[sg].rearrange("p (b t) -> p b t", b=4), ["trs%d" % sg], ["FT"], q="gpsimd")

        for gi in range(NT):
            b = gi % 2
            P = Pt[b]
            kP = "Pt%d" % b
            c0 = gi * 128
            self.dma(P, self.Pall[c0:c0 + 128, 0:NQKV], ["Pall"], [kP])
            self.dma(cs[b], self.Cd["rope"][c0:c0 + 128, :], (), ["cs%d" % b])
            self.act(sq, P[:, 0:1676], AF.Square, [kP], ["sq"])
            self.red(stat[:, 0:10], sq[:, 0:640].rearrange("p (g d) -> p g d", d=64), ALU.add, ["sq"], ["stat"])
            self.red(stat[:, 10:26], sq[:, 652:1676].rearrange("p (g d) -> p g d", d=64), ALU.add, ["sq"], ["stat"])
            self.rsq(stat[:, 0:26], 1.0 / 64, "stat")
            self.mset(stat[:, 4:6], 1.0, ["stat"])
            self.tt(ZA.rearrange("p (g d) -> p g d", d=64), P[:, 0:640].rearrange("p (g d) -> p g d", d=64),
                    stat[:, 0:10].unsqueeze(2).to_broadcast([128, 10, 64]), ALU.mult, [kP, "stat"], ["ZA"])
            self.tt(ZA, ZA, gainA, ALU.mult, ["ZA", "gain"], ["ZA"])
            self.tt(ZB.rearrange("p (g d) -> p g d", d=64), P[:, 652:1676].rearrange("p (g d) -> p g d", d=64),
                    stat[:, 10:26].unsqueeze(2).to_broadcast([128, 16, 64]), ALU.mult, [kP, "stat"], ["ZB"])
            self.tt(ZB, ZB, gainB, ALU.mult, ["ZB", "gain"], ["ZB"])
            tgroup([(ZA[:, i * 128:(i + 1) * 128], "ZA") for i in range(4)], 0, c0)
            tgroup([(ZA[:, 512:640], "ZA")] + [(ZB[:, i * 128:(i + 1) * 128], "ZB") for i in range(3)], 512, c0)
            tgroup([(ZB[:, i * 128:(i + 1) * 128], "ZB") for i in range(4, 8)], 1152, c0)
            self.act(junk, P[:, 1932:2316], AF.Square, [kP], ["junk", "st2"], accum=stat[:, 32:33])
            self.act(junk[:, 0:128], P[:, 2316:2444], AF.Square, [kP], ["junk", "st2"], accum=stat[:, 33:34])
            self.act(junk[:, 0:32], P[:, 2444:2476], AF.Square, [kP], ["junk", "st2"], accum=stat[:, 34:35])
            self.ts(stat[:, 32:33], stat[:, 32:33], 1.0 / 384, EPS, ALU.mult, ALU.add, ["st2"], ["st2"])
            self.ts(stat[:, 33:34], stat[:, 33:34], 1.0 / 128, EPS, ALU.mult, ALU.add, ["st2"], ["st2"])
            self.ts(stat[:, 34:35], stat[:, 34:35], 1.0 / 32, EPS, ALU.mult, ALU.add, ["st2"], ["st2"])
            self.act(stat[:, 32:35], stat[:, 32:35], AF.Sqrt, ["st2"], ["st2"])
            self.rcp(stat[:, 32:35], stat[:, 32:35], ["st2"], ["st2"])
            self.stt(cqn[:, 0:384], P[:, 1932:2316], stat[:, 32:33], lgq, ALU.mult, ALU.mult, [kP, "st2", "lg"], ["cqn"])
            self.stt(cqn[:, 384:512], P[:, 2316:2444], stat[:, 33:34], lgkv, ALU.mult, ALU.mult, [kP, "st2", "lg"], ["cqn"])
            self.stt(krn, P[:, 2444:2476], stat[:, 34:35], gqd[:, 160:192], ALU.mult, ALU.mult, [kP, "st2", "gqd"], ["krn"])
            for i in range(4):
                self.tr(self.bank(4)[:, i * 128:(i + 1) * 128], cqn[:, i * 128:(i + 1) * 128], ["cqn", "ident"], ["ps4"])
            self.cp(cT.rearrange("p k t -> p (k t)"), self.bank(4), ["ps4"], ["cT"], eng="scalar")
            for k in range(3):
                self.mm(self.bank(5)[:, 0:384], cT[:, k, :], wuq[:, k, :], k == 0, k == 2, ["cT", "wu"], ["ps5"])
            self.mm(self.bank(6), cT[:, 3, :], wukv, True, True, ["cT", "wu"], ["ps6"])
            qup = self.bank(5)[:, 0:384].rearrange("p (h d) -> p h d", h=4)
            kvup = self.bank(6).rearrange("p (h d) -> p h d", h=4)
            self.act(sqq, self.bank(5)[:, 0:384], AF.Square, ["ps5"], ["sqq"])
            self.act(sqk.rearrange("p (h d) -> p h d", h=4), kvup[:, :, 0:64], AF.Square, ["ps6"], ["sqk"])
            sq3 = sqq.rearrange("p (h d) -> p h d", h=4)
            self.red(stat[:, 40:44], sq3[:, :, 0:64], ALU.add, ["sqq"], ["st3"])
            self.red(stat[:, 44:48], sqk.rearrange("p (h d) -> p h d", h=4), ALU.add, ["sqk"], ["st3"])
            self.red(stat[:, 48:52], sq3[:, :, 64:96], ALU.add, ["sqq"], ["st3"])
            self.ts(stat[:, 40:48], stat[:, 40:48], 1.0 / 64, EPS, ALU.mult, ALU.add, ["st3"], ["st3"])
            self.ts(stat[:, 48:52], stat[:, 48:52], 1.0 / 32, EPS, ALU.mult, ALU.add, ["st3"], ["st3"])
            self.act(stat[:, 40:52], stat[:, 40:52], AF.Sqrt, ["st3"], ["st3"])
            self.rcp(stat[:, 40:52], stat[:, 40:52], ["st3"], ["st3"])
            self.tt(qf[:, :, 0:64], qup[:, :, 0:64], stat[:, 40:44].unsqueeze(2).to_broadcast([128, 4, 64]), ALU.mult, ["ps5", "st3"], ["qf"])
            self.tt(qf[:, :, 0:64], qf[:, :, 0:64], gqd[:, 0:64].unsqueeze(1).to_broadcast([128, 4, 64]), ALU.mult, ["qf", "gqd"], ["qf"])
            self.tt(kf[:, :, 0:64], kvup[:, :, 0:64], stat[:, 44:48].unsqueeze(2).to_broadcast([128, 4, 64]), ALU.mult, ["ps6", "st3"], ["kf"])
            self.tt(kf[:, :, 0:64], kf[:, :, 0:64], gqd[:, 96:160].unsqueeze(1).to_broadcast([128, 4, 64]), ALU.mult, ["kf", "gqd"], ["kf"])
            self.cp(vd.rearrange("p (h d) -> p h d", h=4), kvup[:, :, 64:128], ["ps6"], ["vd"], eng="scalar")
            self.dma(self.Vd[c0:c0 + 128, :], vd, ["vd"], ["Vd"], q="gpsimd")
            q3 = qr.rearrange("p (h d) -> p h d", h=4)
            self.tt(q3, qup[:, :, 64:96], stat[:, 48:52].unsqueeze(2).to_broadcast([128, 4, 32]), ALU.mult, ["ps5", "st3"], ["qr"])
            self.tt(q3, q3, gqd[:, 64:96].unsqueeze(1).to_broadcast([128, 4, 32]), ALU.mult, ["qr", "gqd"], ["qr"])
            cosb = cs[b][:, 0:16].unsqueeze(1).to_broadcast([128, 4, 16])
            sinb = cs[b][:, 16:32].unsqueeze(1).to_broadcast([128, 4, 16])
            t0 = tmp[0].rearrange("p (h d) -> p h d", h=4)
            t1 = tmp[1].rearrange("p (h d) -> p h d", h=4)
            kc = "cs%d" % b
            self.tt(t0, q3[:, :, 0:16], cosb, ALU.mult, ["qr", kc], ["t0"])
            self.tt(t1, q3[:, :, 16:32], sinb, ALU.mult, ["qr", kc], ["t1"])
            self.tt(qf[:, :, 64:80], t0, t1, ALU.subtract, ["t0", "t1"], ["qf"])
            self.tt(t0, q3[:, :, 0:16], sinb, ALU.mult, ["qr", kc], ["t0"])
            self.tt(t1, q3[:, :, 16:32], cosb, ALU.mult, ["qr", kc], ["t1"])
            self.tt(qf[:, :, 80:96], t0, t1, ALU.add, ["t0", "t1"], ["qf"])
            c1 = cs[b][:, 0:16]
            s1 = cs[b][:, 16:32]
            self.tt(tmp[2][:, 0:16], krn[:, 0:16], c1, ALU.mult, ["krn", kc], ["t2"])
            self.tt(tmp[3][:, 0:16], krn[:, 16:32], s1, ALU.mult, ["krn", kc], ["t3"])
            self.tt(krot[:, 0:16], tmp[2][:, 0:16], tmp[3][:, 0:16], ALU.subtract, ["t2", "t3"], ["krot"])
            self.tt(tmp[2][:, 0:16], krn[:, 0:16], s1, ALU.mult, ["krn", kc], ["t2"])
            self.tt(tmp[3][:, 0:16], krn[:, 16:32], c1, ALU.mult, ["krn", kc], ["t3"])
            self.tt(krot[:, 16:32], tmp[2][:, 0:16], tmp[3][:, 0:16], ALU.add, ["t2", "t3"], ["krot"])
            self.cp(kf[:, :, 64:96], krot.unsqueeze(1).to_broadcast([128, 4, 32]), ["krot"], ["kf"])
            tgroup([(qf[:, h, :], "qf") for h in range(4)], 1664, c0)
            tgroup([(kf[:, h, :], "kf") for h in range(4)], 2176, c0)

    def load_v(self, V, src_ap, nh, key):
        V4 = V.rearrange("p (k h d) -> p k h d", k=32, h=nh)
        self.mset(V, 1.0, [key])
        for c in range(4):
            for h in range(nh):
                self.dma(V4[:, c * 8:(c + 1) * 8, h, 0:64],
                         src_ap[c * 1024:(c + 1) * 1024, h * 64:(h + 1) * 64].rearrange("(k p) d -> p k d", p=128), ["Pall", "Vd"], [key])
        return V4

    def ph_nsa(self, l):
        self.reset()
        FT = self.FT
        raw = self.A(4096)
        w1c = self.A(8192).rearrange("p (r m) -> p r m", r=32)
        pe32 = self.A(128, 32)
        peT = self.A(32)
        cpe = self.A(4)
        hid = [[self.A(256) for _ in range(2)] for _ in range(2)]
        w2c = self.A(256).rearrange("p (s c d) -> p s c d", s=2, c=2)
        kcn = self.A(128).rearrange("p (c d) -> p c d", c=2)
        sqc = self.A(128)
        kcT = self.A(256)
        vcx = self.A(258).rearrange("p (c d) -> p c d", c=2)
        KsT = self.A(4096)
        KwT = self.A(4096)
        Vs = self.A(32 * 65)
        Vw = self.A(32 * 65)
        Ebf = self.A(2048, 64).bitcast(BF16)
        mcmp = self.A(4096).bitcast(BF16).rearrange("p (c t) -> p c t", c=2)
        triA = self.A(256).bitcast(BF16)
        triB = self.A(256).bitcast(BF16)
        qa = [self.A(512), self.A(512)]
        gl = [self.A(12), self.A(12)]
        selc = [self.A(192), self.A(192)]
        PT = [self.A(512) for _ in range(3)]
        oa = self.A(256)
        stat = self.A(64)
        score = self.A(64)
        top8 = self.A(8)
        selb = self.A(64)
        selbT = self.A(256, 64).bitcast(BF16)
        otsb = self.A(512)
        atsb = self.A(512)
        Cd = self.Cd
        self.dma(Ebf, Cd["E"], (), ["Ebf"])
        self.dma(mcmp, Cd["mcmp"], (), ["mcmp"])
        self.dma(triA, Cd["triA"], (), ["tri"])
        self.dma(triB, Cd["triB"], (), ["tri"])
        self.dma(raw, FT[256:384, :], ["FT"], ["raw"])
        for s in range(2):
            self.dma(w1c[64 * s:64 * s + 64], self.Wd["cmp_w1"][l, s].rearrange("(r d) m -> d r m", d=64), (), ["w1c"])
            self.dma(pe32[:, 64 * s:64 * s + 64], self.Wd["cmp_pe"][l, s], (), ["pe32"])
        self.dma(w2c, self.Wd["cmp_w2"][l].rearrange("s (c p) d -> p s c d", p=128), (), ["w2c"])
        self.dma(KsT[0:64], FT[384:448, :], ["FT"], ["KsT"])
        self.dma(KsT[64:66], Cd["kaug"], (), ["KsT"])
        self.dma(KwT[0:64], FT[512:576, :], ["FT"], ["KwT"])
        self.dma(KwT[64:66], Cd["kaug"], (), ["KwT"])
        Vs4 = self.load_v(Vs, self.Pall[:, 448:512], 1, "Vs")
        Vw4 = self.load_v(Vw, self.Pall[:, 576:640], 1, "Vw")
        self.tr(self.bank(7)[:, 0:32], pe32, ["pe32", "ident"], ["ps7"])
        self.cp(peT, self.bank(7)[:, 0:32], ["ps7"], ["peT"])
        for s in range(2):
            sp = slice(64 * s, 64 * s + 64)
            for mc in range(2):
                col = s * 2 + mc
                for r in range(32):
                    self.mm(self.bank(4 + col)[:, 0:1], w1c[sp, r, mc * 128:(mc + 1) * 128], peT[sp, r:r + 1], r == 0, r == 31, ["w1c", "peT"], ["ps%d" % (4 + col)])
                self.cp(cpe[:, col:col + 1], self.bank(4 + col)[:, 0:1], ["ps%d" % (4 + col)], ["cpe"])
        for s in range(2):
            sp = slice(64 * s, 64 * s + 64)
            for mc in range(2):
                bk = (s * 2 + mc) % 2
                for r in range(32):
                    self.mm(self.bank(bk)[:, 0:255], w1c[sp, r, mc * 128:(mc + 1) * 128], raw[sp, r:r + 16 * 254 + 1:16], r == 0, r == 31, ["w1c", "raw"], ["ps%d" % bk])
                hk = "hid%d%d" % (s, mc)
                self.mset(hid[s][mc], 0.0, [hk])
                self.act(hid[s][mc][:, 0:255], self.bank(bk)[:, 0:255], AF.Silu, ["ps%d" % bk, "cpe"], [hk], bias=cpe[:, s * 2 + mc:s * 2 + mc + 1])
        self.mset(vcx.rearrange("p c d -> p (c d)"), 1.0, ["vcx"])
        self.dma(vcx[:, :, 65:129], Cd["cover"], (), ["vcx"])
        for s in range(2):
            for ch in range(2):
                bk = 2 + ch
                for mc in range(2):
                    self.mm(self.bank(bk)[:, 0:64], hid[s][mc][:, ch * 128:(ch + 1) * 128], w2c[:, s, mc, :], mc == 0, mc == 1,
                            ["hid%d%d" % (s, mc), "w2c"], ["ps%d" % bk])
                if s == 0:
                    self.cp(kcn[:, ch, :], self.bank(bk)[:, 0:64], ["ps%d" % bk], ["kcn"])
                else:
                    self.cp(vcx[:, ch, 0:64], self.bank(bk)[:, 0:64], ["ps%d" % bk], ["vcx"])
        kc2 = kcn.rearrange("p c d -> p (c d)")
        self.tt(sqc, kc2, kc2, ALU.mult, ["kcn"], ["sqc"])
        self.red(stat[:, 0:2], sqc.rearrange("p (c d) -> p c d", c=2), ALU.add, ["sqc"], ["stat"])
        self.rsq(stat[:, 0:2], 1.0 / 64, "stat")
        self.tt(kcn, kcn, stat[:, 0:2].unsqueeze(2).to_broadcast([128, 2, 64]), ALU.mult, ["kcn", "stat"], ["kcn"])
        self.tt(kcn, kcn, self.gq[:, 64:128].unsqueeze(1).to_broadcast([128, 2, 64]), ALU.mult, ["kcn", "gq"], ["kcn"])
        for ch in range(2):
            self.tr(self.bank(7)[0:64, ch * 128:(ch + 1) * 128], kcn[:, ch, :], ["kcn", "ident"], ["ps7"])
        self.cp(kcT[0:64], self.bank(7)[0:64, 0:256], ["ps7"], ["kcT"])
        self.dma(kcT[64:66], Cd["kaugc"], (), ["kcT"])
        identb = self.identb
        if self.debug:
            self.dma(self.dbg3, kcn, ["kcn"], ["dbg3"], q="gpsimd")
            self.dma(self.dbg4, vcx, ["vcx"], ["dbg4"], q="gpsimd")
            self.dma(self.dbg5, kcT[0:66], ["kcT"], ["dbg5"], q="gpsimd")
        PTc = [self.A(512), self.A(512)]
        otsbC = self.A(512)
        oas = [oa, self.A(256)]
        selbTs = [selbT, self.A(256, 64).bitcast(BF16)]
        statC = [self.A(32), self.A(32)]

        def cmp_stage(i):
            b = i % 2
            c0 = i * 128
            q = qa[b]
            kq = "qa%d" % b
            q3 = q.rearrange("p (h t) -> p h t", h=4)
            st = statC[b]
            ks = "stC%d" % b
            ko = "oa%d" % b
            self.dma(q3[0:64], FT[0:256, c0:c0 + 128].rearrange("(h d) t -> d h t", d=64), ["FT"], [kq])
            self.dma(q3[64:66], Cd["qaug"][0][:, :, c0:c0 + 128], (), [kq])
            self.dma(gl[b], self.Pall[c0:c0 + 128, 640:652], ["Pall"], ["gl%d" % b])
            self.dma(selc[b], Cd["selc"][c0:c0 + 128].rearrange("p a j -> p (a j)"), (), ["selc%d" % b])
            g12 = gl[b]
            self.act(g12, g12, AF.Sigmoid, ["gl%d" % b], ["gl%d" % b])
            g3 = g12.rearrange("p (h k) -> p h k", k=3)
            nnt = 1 if i < 16 else 2
            for nt in range(nnt):
                self.mm(self.bank(6), kcT[0:66, nt * 128:(nt + 1) * 128], q[0:66, :], True, False, ["kcT", kq], ["ps6"])
                self.mm(self.bank(6).rearrange("p (h t) -> p h t", h=4), identb, mcmp[:, nt, c0:c0 + 128].unsqueeze(1).to_broadcast([128, 4, 128]),
                        False, True, ["identb", "mcmp"], ["ps6"])
                self.act(PTc[nt], self.bank(6), AF.Exp, ["ps6"], ["PTc%d" % nt])
            for nt in range(nnt):
                self.mm(self.bank(3)[0:65, :], vcx[:, nt, 0:65], PTc[nt], nt == 0, nt == nnt - 1, ["PTc%d" % nt, "vcx"], ["ps3"])
            for nt in range(nnt):
                self.mm(self.bank(5)[0:64, :], vcx[:, nt, 65:129], PTc[nt], nt == 0, nt == nnt - 1, ["PTc%d" % nt, "vcx"], ["ps5"])
            self.cp(otsbC[0:65, :], self.bank(3)[0:65, :], ["ps3"], ["otsbC"], eng="scalar")
            self.cp(atsb[0:64, :], self.bank(5)[0:64, :], ["ps5"], ["atsb"])

        def cmp_stageB(i):
            b = i % 2
            st = statC[b]
            ks = "stC%d" % b
            ko = "oa%d" % b
            g3 = gl[b].rearrange("p (h k) -> p h k", k=3)
            for h in range(4):
                self.tr(self.bank(7)[:, h * 65:(h + 1) * 65], otsbC[0:65, h * 128:(h + 1) * 128], ["otsbC", "ident"], ["ps7"])
                self.tr(self.bank(6)[:, h * 64:(h + 1) * 64], atsb[0:64, h * 128:(h + 1) * 128], ["atsb", "ident"], ["ps6"])

            def creg(h):
                return self.bank(7)[:, h * 65:(h + 1) * 65]

            def areg(h):
                return self.bank(6)[:, h * 64:(h + 1) * 64]

            for h in range(4):
                self.ts(st[:, 8 + h:9 + h], creg(h)[:, 64:65], 1e-30, None, ALU.max, None, ["ps7"], [ks])
            self.rcp(st[:, 8:12], st[:, 8:12], [ks], [ks])
            self.tt(st[:, 12:16], st[:, 8:12], g3[:, :, 0], ALU.mult, [ks, "gl%d" % b], [ks])
            oa3 = oas[b].rearrange("p (h d) -> p h d", h=4)
            for h in range(4):
                self.ts(oa3[:, h, :], creg(h)[:, 0:64], st[:, 12 + h:13 + h], None, ALU.mult, None, ["ps7", ks], [ko])
            self.ts(score, areg(0), st[:, 8:9], None, ALU.mult, None, ["ps6", ks], ["score"])
            for h in range(1, 4):
                self.stt(score, areg(h), st[:, 8 + h:9 + h], score, ALU.mult, ALU.add, ["ps6", ks, "score"], ["score"])
            sc3 = selc[b].rearrange("p (a j) -> p a j", a=3)
            ksc = "selc%d" % b
            self.tt(score, score, sc3[:, 0, :], ALU.add, ["score", ksc], ["score"])
            self.tt(score, score, sc3[:, 1, :], ALU.min, ["score", ksc], ["score"])
            self.vmax(top8, score, ["score"], ["top8"])
            self.ts(selb, score, top8[:, 7:8], None, ALU.is_ge, None, ["score", "top8"], ["selb"])
            self.ts(selb, selb, -NEG, NEG, ALU.mult, ALU.add, ["selb"], ["selb"])
            self.tt(selb, selb, sc3[:, 2, :], ALU.add, ["selb", ksc], ["selb"])

        def cmp_stageC(i):
            b = i % 2
            self.tr(self.bank(7)[0:64, 384:512], selb, ["selb", "ident"], ["ps7"])
            self.cp(selbTs[b].rearrange("p (h t) -> p h t", h=4), self.bank(7)[0:64, 384:512].unsqueeze(1).to_broadcast([64, 4, 128]), ["ps7"], ["selbT%d" % b])

        def br_stage(i, hooks):
            b = i % 2
            c0 = i * 128
            q = qa[b]
            kq = "qa%d" % b
            ko = "oa%d" % b
            g3 = gl[b].rearrange("p (h k) -> p h k", k=3)
            oa3 = oas[b].rearrange("p (h d) -> p h d", h=4)
            sT = selbTs[b]
            ksT = "selbT%d" % b

            def breg(h):
                return self.bank(4)[:, h * 65:(h + 1) * 65]

            for br in (1, 2):
                kts = list(range(0, i + 1)) if br == 1 else list(range(max(0, i - 4), i + 1))
                KT = KsT if br == 1 else KwT
                kk = "KsT" if br == 1 else "KwT"
                V4 = Vs4 if br == 1 else Vw4
                kv = "Vs" if br == 1 else "Vw"
                nk_ = len(kts)
                for n_ in range(nk_ + 1):
                    if n_ < nk_:
                        kt = kts[n_]
                        bk = n_ % 2
                        pt = n_ % 3
                        pk = "ps%d" % bk
                        last_extra = (kt == i) or (br == 2 and kt == i - 4)
                        self.mm(self.bank(bk), KT[0:66, kt * 128:(kt + 1) * 128], q[0:66, :], True, (br == 2 and not last_extra), [kk, kq], [pk])
                        if br == 1:
                            self.mm(self.bank(bk), Ebf[:, kt * 128:(kt + 1) * 128], sT, False, not last_extra, ["Ebf", ksT], [pk])
                        if kt == i:
                            self.mm(self.bank(bk), identb, triA, False, True, ["identb", "tri"], [pk])
                        elif br == 2 and kt == i - 4:
                            self.mm(self.bank(bk), identb, triB, False, True, ["identb", "tri"], [pk])
                        self.act(PT[pt], self.bank(bk), AF.Exp, [pk], ["PT%d" % pt])
                    if n_ >= 1:
                        m_ = n_ - 1
                        self.mm(self.bank(2)[0:65, :], V4[:, kts[m_], 0, :], PT[m_ % 3], m_ == 0, m_ == nk_ - 1, ["PT%d" % (m_ % 3), kv], ["ps2"])
                self.cp(otsb[0:65, :], self.bank(2)[0:65, :], ["ps2"], ["otsb"], eng="scalar")
                for h in range(4):
                    self.tr(self.bank(4)[:, h * 65:(h + 1) * 65], otsb[0:65, h * 128:(h + 1) * 128], ["otsb", "ident"], ["ps4"])
                for h in range(4):
                    self.cp(stat[:, 16 + h:17 + h], breg(h)[:, 64:65], ["ps4"], ["st"])
                self.rcp(stat[:, 16:20], stat[:, 16:20], ["st"], ["st"])
                self.tt(stat[:, 20:24], stat[:, 16:20], g3[:, :, br], ALU.mult, ["st", "gl%d" % b], ["st"])
                for h in range(4):
                    self.stt(oa3[:, h, :], breg(h)[:, 0:64], stat[:, 20 + h:21 + h], oa3[:, h, :], ALU.mult, ALU.add,
                             ["ps4", "st", ko], [ko])
                if hooks:
                    hooks[br - 1]()
            self.dma(self.Od[c0:c0 + 128, 0:256], oas[b], [ko], ["Od"], q="gpsimd")

        cmp_stage(0)
        cmp_stageB(0)
        cmp_stageC(0)
        for i in range(NT):
            if i + 1 < NT:
                cmp_stage(i + 1)
                br_stage(i, [lambda j=i + 1: cmp_stageB(j), lambda j=i + 1: cmp_stageC(j)])
            else:
                br_stage(i, None)

    def ph_swa(self, l):
        self.reset()
        FT = self.FT
        Cd = self.Cd
        KTb = self.A(2 * 4096).rearrange("p (g t) -> p g t", g=2)
        Vb = self.A(32 * 2 * 65)
        triA = self.A(256).bitcast(BF16)
        triB = self.A(256).bitcast(BF16)
        qb = [self.A(512), self.A(512)]
        PT = [self.A(512) for _ in range(3)]
        ob = self.A(256)
        stat = self.A(16)
        otsb = self.A(512)
        identb = self.identb
        self.dma(triA, Cd["triA"], (), ["tri"])
        self.dma(triB, Cd["triB"], (), ["tri"])
        self.dma(KTb[0:64], FT[896:1024, :].rearrange("(g d) t -> d g t", d=64), ["FT"], ["KTb"])
        for g in range(2):
            self.dma(KTb[64:66, g, :], Cd["kaug"], (), ["KTb"])
        Vb4 = self.load_v(Vb, self.Pall[:, 1036:1164], 2, "Vb")
        self.dma(self.esink, self._bc_row(self.Wd["swa_sinks"][l:l + 1, :], 4), (), ["esink"])
        self.act(self.esink, self.esink, AF.Exp, ["esink"], ["esink"])
        ob3 = ob.rearrange("p (h d) -> p h d", h=4)
        for i in range(NT):
            b = i % 2
            c0 = i * 128
            q = qb[b]
            kq = "qb%d" % b
            q3 = q.rearrange("p (h t) -> p h t", h=4)
            self.dma(q3[0:64], FT[640:896, c0:c0 + 128].rearrange("(h d) t -> d h t", d=64), ["FT"], [kq])
            self.dma(q3[64:66], Cd["qaug"][1][:, :, c0:c0 + 128], (), [kq])
            kts = [kt for kt in (i - 1, i) if kt >= 0]
            for n_, kt in enumerate(kts):
                bk = (i + n_) % 2
                pt = (i + n_) % 3
                pk = "ps%d" % bk
                tri = triA if kt == i else triB
                for g in range(2):
                    reg = self.bank(bk)[:, g * 256:(g + 1) * 256]
                    self.mm(reg, KTb[0:66, g, kt * 128:(kt + 1) * 128], q[0:66, g * 256:(g + 1) * 256], True, False, ["KTb", kq], [pk])
                    self.mm(reg, identb, tri[:, 0:256], False, True, ["identb", "tri"], [pk])
                self.act(PT[pt], self.bank(bk), AF.Exp, [pk], ["PT%d" % pt])
                for g in range(2):
                    self.mm(self.bank(2 + g)[0:65, 0:256], Vb4[:, kt, g, :], PT[pt][:, g * 256:(g + 1) * 256], n_ == 0, n_ == len(kts) - 1,
                            ["PT%d" % pt, "Vb"], ["ps%d" % (2 + g)])
            for g in range(2):
                self.cp(otsb[0:65, g * 256:(g + 1) * 256], self.bank(2 + g)[0:65, 0:256], ["ps%d" % (2 + g)], ["otsb"], eng=("scalar" if g == 0 else "vector"))
            for h in range(4):
                self.tr(self.bank(4)[:, h * 65:(h + 1) * 65], otsb[0:65, h * 128:(h + 1) * 128], ["otsb", "ident"], ["ps4"])
            for h in range(4):
                self.tt(stat[:, h:h + 1], self.bank(4)[:, h * 65 + 64:h * 65 + 65], self.esink[:, h:h + 1], ALU.add, ["ps4", "esink"], ["st"])
            self.rcp(stat[:, 0:4], stat[:, 0:4], ["st"], ["st"])
            for h in range(4):
                self.ts(ob3[:, h, :], self.bank(4)[:, h * 65:h * 65 + 64], stat[:, h:h + 1], None, ALU.mult, None, ["ps4", "st"], ["ob"])
            self.dma(self.Od[c0:c0 + 128, 256:512], ob, ["ob"], ["Od"], q="gpsimd")

    def ph_dense(self, l, moba):
        self.reset()
        FT = self.FT
        Cd = self.Cd
        KR = 66 if moba else 96
        KT = self.A(4 * 4096).rearrange("p (h t) -> p h t", h=4)
        V = self.A(32 * 4 * 65)
        t512 = self.A(1024).bitcast(BF16).rearrange("p (j t) -> p j t", j=4)
        qg = [self.A(2048), self.A(2048)]
        PT = [self.A(512) for _ in range(3)]
        oc = self.A(1024).rearrange("p (j h d) -> p j h d", j=4, h=4)
        stat = self.A(16)
        otsb = [self.A(512), self.A(512)]
        cnt = 0
        ptl = []
        identb = self.identb
        self.dma(t512, Cd["t512"], (), ["t512"])
        if moba:
            Embf = self.A(4096, 64).bitcast(BF16).rearrange("p (v k) -> p v k", v=64)
            kmT = self.A(64, 64).rearrange("p (h j) -> p h j", h=4)
            mcst = [self.A(48), self.A(48)]
            gsm = self.A(64)
            t8 = self.A(32)
            selb = self.A(64)
            selbT = self.A(256, 64).bitcast(BF16)
            self.dma(Embf, Cd["Em"], (), ["Embf"])
            self.dma(KT[0:64], FT[1408:1664, :].rearrange("(h d) t -> d h t", d=64), ["FT"], ["KT"])
            for h in range(4):
                self.dma(KT[64:66, h, :], Cd["kaug"], (), ["KT"])
            V4 = self.load_v(V, self.Pall[:, 1676:1932], 4, "V")
            self.red(kmT, KT[0:64].rearrange("p h (j t) -> p h j t", j=16), ALU.add, ["KT"], ["kmT"])
            ocol = 512
            qrow = 1152
        else:
            self.dma(KT[0:96], FT[2176:2688, :].rearrange("(h r) t -> r h t", r=128)[0:96], ["FT"], ["KT"])
            V4 = self.load_v(V, self.Vd, 4, "V")
            ocol = 768
            qrow = 1664
        for g in range(8):
            b = g % 2
            q = qg[b]
            kq = "qg%d" % b
            q3 = q.rearrange("p (h t) -> p h t", h=4)
            g0 = g * 512
            if moba:
                self.dma(q3[0:64], FT[qrow:qrow + 256, g0:g0 + 512].rearrange("(h d) t -> d h t", d=64), ["FT"], [kq])
                self.dma(q3[64:66], Cd["qaug"][2][:, :, g0:g0 + 512], (), [kq])
                for j in range(4):
                    mb = j % 2
                    c0 = g0 + j * 128
                    self.dma(mcst[mb], Cd["mconst"][c0:c0 + 128].rearrange("p a j -> p (a j)"), (), ["mc%d" % mb])
                    m3 = mcst[mb].rearrange("p (a j) -> p a j", a=3)
                    for h in range(4):
                        self.mm(self.bank(6)[:, h * 16:(h + 1) * 16], q3[0:64, h, j * 128:(j + 1) * 128], kmT[:, h, :], True, True, [kq, "kmT"], ["ps6"])
                    gs3 = gsm.rearrange("p (h j) -> p h j", h=4)
                    self.tt(gs3, self.bank(6)[:, 0:64].rearrange("p (h j) -> p h j", h=4), m3[:, 0, :].unsqueeze(1).to_broadcast([128, 4, 16]),
                            ALU.min, ["ps6", "mc%d" % mb], ["gsm"])
                    for h in range(4):
                        self.vmax(t8[:, h * 8:(h + 1) * 8], gsm[:, h * 16:(h + 1) * 16], ["gsm"], ["t8"])
                    s3 = selb.rearrange("p (h j) -> p h j", h=4)
                    for h in range(4):
                        self.ts(s3[:, h, :], gs3[:, h, :], t8[:, h * 8 + 2:h * 8 + 3], None, ALU.is_ge, None, ["gsm", "t8"], ["selb"])
                    self.tt(s3, s3, m3[:, 1, :].unsqueeze(1).to_broadcast([128, 4, 16]), ALU.mult, ["selb", "mc%d" % mb], ["selb"])
                    self.tt(s3, s3, m3[:, 2, :].unsqueeze(1).to_broadcast([128, 4, 16]), ALU.add, ["selb", "mc%d" % mb], ["selb"])
                    self.ts(selb, selb, -NEG, NEG, ALU.mult, ALU.add, ["selb"], ["selb"])
                    self.tr(self.bank(7)[0:64, 0:128], selb, ["selb", "ident"], ["ps7"])
                    self.cp(selbT[:, j * 128:(j + 1) * 128], self.bank(7)[0:64, 0:128], ["ps7"], ["selbT"])
            else:
                self.dma(q3[0:96], FT[qrow:qrow + 512, g0:g0 + 512].rearrange("(h r) t -> r h t", r=128)[0:96], ["FT"], [kq])
            nk = 4 * g + 4
            for h in range(4):
                ab = 2 + h % 2
                tb = 4 + h % 2
                for n_ in range(nk + 1):
                    if n_ < nk:
                        kt = n_
                        bk = cnt % 2
                        pt = cnt % 3
                        pk = "ps%d" % bk
                        diag = kt >= 4 * g
                        self.mm(self.bank(bk), KT[0:KR, h, kt * 128:(kt + 1) * 128], q3[0:KR, h, :], True, (not moba) and (not diag), ["KT", kq], [pk])
                        if moba:
                            self.mm(self.bank(bk), Embf[:, h * 16 + kt // 2, :], selbT, False, not diag, ["Embf", "selbT"], [pk])
                        if diag:
                            self.mm(self.bank(bk), identb, t512[:, kt - 4 * g, :], False, True, ["identb", "t512"], [pk])
                        self.act(PT[pt], self.bank(bk), AF.Exp, [pk], ["PT%d" % pt])
                        ptl.append(pt)
                        cnt += 1
                    if n_ >= 1:
                        m_ = n_ - 1
                        pm = ptl[len(ptl) - 1 - (1 if n_ < nk else 0)]
                        self.mm(self.bank(ab)[0:65, :], V4[:, m_, h, :], PT[pm], m_ == 0, m_ == nk - 1, ["PT%d" % pm, "V"], ["ps%d" % ab])
                so = otsb[h % 2]
                ko = "otsb%d" % (h % 2)
                self.cp(so[0:65, :], self.bank(ab)[0:65, :], ["ps%d" % ab], [ko], eng=("scalar" if h % 2 == 0 else "vector"))
                for j in range(4):
                    self.tr(self.bank(tb)[:, j * 65:(j + 1) * 65], so[0:65, j * 128:(j + 1) * 128], [ko, "ident"], ["ps%d" % tb])
                for j in range(4):
                    self.cp(stat[:, j:j + 1], self.bank(tb)[:, j * 65 + 64:j * 65 + 65], ["ps%d" % tb], ["st"])
                self.rcp(stat[:, 0:4], stat[:, 0:4], ["st"], ["st"])
                for j in range(4):
                    self.ts(oc[:, j, h, :], self.bank(tb)[:, j * 65:j * 65 + 64], stat[:, j:j + 1], None, ALU.mult, None, ["ps%d" % tb, "st"], ["oc"])
            for j in range(4):
                c0 = g0 + j * 128
                self.dma(self.Od[c0:c0 + 128, ocol:ocol + 256], oc[:, j].rearrange("p h d -> p (h d)"), ["oc"], ["Od"], q="gpsimd")

    def ph_merge(self, l, xin):
        self.reset()
        wbr = self.A(8192).rearrange("p (n k d) -> p n k d", n=4, k=2)
        wout = self.A(8192).rearrange("p (k d) -> p k d", k=8)
        Gt = [self.A(4096), self.A(4096)]
        Ot = [self.A(1024), self.A(1024)]
        OT = self.A(1024).rearrange("p (k t) -> p k t", k=8)
        mp = self.A(1024)
        mpT = self.A(1024).rearrange("p (k t) -> p k t", k=8)
        xt = [self.A(1024), self.A(1024)]
        tmp = self.A(512)
        xo = [self.A(1024), self.A(1024)]
        self.dma(wbr, self.Wl("w_branch", l).rearrange("n (k p) d -> p n k d", p=128), (), ["wbr"])
        self.dma(wout, self.Wl("w_out", l).rearrange("(k p) d -> p k d", p=128), (), ["wout"])
        cnt = 0
        for i in range(NT):
            b = i % 2
            c0 = i * 128
            self.dma(Ot[b], self.Od[c0:c0 + 128, :], ["Od"], ["Ot%d" % b])
            self.dma(Gt[b], self.Pall[c0:c0 + 128, NQKV:DIN], ["Pall"], ["Gt%d" % b])
            self.dma(xt[b], xin[c0:c0 + 128, :], (), ["xt%d" % b])
            self.act(Gt[b], Gt[b], AF.Sigmoid, ["Gt%d" % b], ["Gt%d" % b])
            self.transpose8(Ot[b], OT, 0, "Ot%d" % b, "OT", i)
            for cc in range(2):
                for n in range(4):
                    bk = cnt % 4
                    cnt += 1
                    pk = "ps%d" % bk
                    for k in range(2):
                        self.mm(self.bank(bk), OT[:, 2 * n + k, :], wbr[:, n, k, cc * 512:(cc + 1) * 512], k == 0, k == 1, ["OT", "wbr"], [pk])
                    gsl = Gt[b][:, n * 1024 + cc * 512:n * 1024 + (cc + 1) * 512]
                    if n == 0:
                        self.tt(mp[:, cc * 512:(cc + 1) * 512], self.bank(bk), gsl, ALU.mult, [pk, "Gt%d" % b], ["mp"])
                    else:
                        self.tt(tmp, self.bank(bk), gsl, ALU.mult, [pk, "Gt%d" % b], ["tmp"])
                        self.tt(mp[:, cc * 512:(cc + 1) * 512], mp[:, cc * 512:(cc + 1) * 512], tmp, ALU.add, ["mp", "tmp"], ["mp"])
            self.transpose8(mp, mpT, 0, "mp", "mpT", i + 1)
            for cc in range(2):
                bk = cnt % 4
                cnt += 1
                pk = "ps%d" % bk
                for k in range(8):
                    self.mm(self.bank(bk), mpT[:, k, :], wout[:, k, cc * 512:(cc + 1) * 512], k == 0, k == 7, ["mpT", "wout"], [pk])
                self.tt(tmp, self.bank(bk), self.mod[:, 2048 + cc * 512:2048 + (cc + 1) * 512], ALU.mult, [pk, "mod"], ["tmp"])
                self.tt(xo[b][:, cc * 512:(cc + 1) * 512], xt[b][:, cc * 512:(cc + 1) * 512], tmp, ALU.add, ["xt%d" % b, "tmp"], ["xo%d" % b])
            self.dma(self.xmid[c0:c0 + 128, :], xo[b], ["xo%d" % b], ["xmid"], q="gpsimd")

    def ph_moe(self, l, xout):
        self.reset()
        h2T = self.A(4096).rearrange("p (k t) -> p k t", k=8)
        acc = self.A(4096).rearrange("p (j d) -> p j d", j=4)
        xm = self.A(4096).rearrange("p (j d) -> p j d", j=4)
        ht = [self.A(1024), self.A(1024)]
        junk = self.A(1024)
        w13b = [self.A(4096), self.A(4096)]
        w2b = [self.A(2048), self.A(2048)]
        actT = [self.A(1024), self.A(1024)]
        sil = self.A(512)
        wr = self.A(8 * 36).rearrange("p (k n) -> p k n", k=8)
        brb = self.A(36)
        lg = self.A(36)
        Wr = self.A(128).rearrange("p (j e) -> p j e", j=4)
        stat = self.A(32)
        top8 = self.A(8)
        lfm = self.A(32)
        sel = self.A(32)
        we = self.A(32)
        oh = self.A(4)
        gm = self.A(32)
        xo = [self.A(1024), self.A(1024)]
        self.dma(wr[:, :, 0:4], self.Wl("w_coarse", l).rearrange("(k p) n -> p k n", p=128), (), ["wr"])
        self.dma(wr[:, :, 4:36], self.Wl("w_fine", l).rearrange("(k p) n -> p k n", p=128), (), ["wr"])
        self.dma(brb[:, 0:4], self._bc_row(self.Wd["b_coarse"][l:l + 1, :], 4), (), ["brb"])
        self.dma(brb[:, 4:36], self._bc_row(self.Wd["b_fine"][l:l + 1, :], 32), (), ["brb"])
        w13 = self.Wd["w13"][l]
        w2 = self.Wd["w2"][l]
        ecnt = 0
        ycnt = 0
        for g in range(8):
            for j in range(4):
                b = j % 2
                c0 = g * 512 + j * 128
                self.dma(xm[:, j, :], self.xmid[c0:c0 + 128, :], ["xmid"], ["xm%d" % j])
                self.norm_tile(xm[:, j, :], ht[b], junk, stat, j, 4096, 3072, "xm%d" % j, "ht%d" % b)
                self.transpose8(ht[b], h2T, j * 128, "ht%d" % b, "h2T", j)
                for k in range(8):
                    self.mm(self.bank(5)[:, 0:36], h2T[:, k, j * 128:(j + 1) * 128], wr[:, k, :], k == 0, k == 7, ["h2T", "wr"], ["ps5"])
                self.tt(lg, self.bank(5)[:, 0:36], brb, ALU.add, ["ps5", "brb"], ["lg"])
                self.red(stat[:, 8:9], lg[:, 0:4], ALU.max, ["lg"], ["rs"])
                self.ts(oh, lg[:, 0:4], stat[:, 8:9], None, ALU.is_ge, None, ["lg", "rs"], ["oh"])
                self.ts(stat[:, 9:10], stat[:, 8:9], -1.0, None, ALU.mult, None, ["rs"], ["rs"])
                self.act(junk[:, 0:4], lg[:, 0:4], AF.Exp, ["lg", "rs"], ["junk", "rs"], bias=stat[:, 9:10], accum=stat[:, 10:11])
                self.ts(gm.rearrange("p (g e) -> p g e", g=4), oh.unsqueeze(2).to_broadcast([128, 4, 8]), 1e9, -1e9, ALU.mult, ALU.add, ["oh"], ["gm"])
                self.tt(lfm, lg[:, 4:36], gm, ALU.add, ["lg", "gm"], ["lfm"])
                self.vmax(top8, lfm, ["lfm"], ["top8"])
                self.ts(sel, lfm, top8[:, 1:2], None, ALU.is_ge, None, ["lfm", "top8"], ["sel"])
                self.ts(stat[:, 11:12], top8[:, 0:1], -1.0, None, ALU.mult, None, ["top8"], ["rs"])
                self.act(we, lfm, AF.Exp, ["lfm", "rs"], ["we"], bias=stat[:, 11:12])
                self.tt(we, we, sel, ALU.mult, ["we", "sel"], ["we"])
                self.red(stat[:, 12:13], we, ALU.add, ["we"], ["rs"])
                self.tt(stat[:, 13:14], stat[:, 12:13], stat[:, 10:11], ALU.mult, ["rs"], ["rs"])
                self.rcp(stat[:, 13:14], stat[:, 13:14], ["rs"], ["rs"])
                self.ts(Wr[:, j, :], we, stat[:, 13:14], None, ALU.mult, None, ["we", "rs"], ["Wr"])
            for e in range(32):
                b = ecnt % 2
                ecnt += 1
                w13v = w13b[b].rearrange("p (k n) -> p k n", k=8)
                w2v = w2b[b].rearrange("p (k d) -> p k d", k=2)
                self.dma(w13v, w13[e].rearrange("(k p) n -> p k n", p=128), (), ["w13b%d" % b])
                self.dma(w2v, w2[e].rearrange("(k p) d -> p k d", p=128), (), ["w2b%d" % b])
                for m in range(4):
                    for k in range(8):
                        self.mm(self.bank(m), w13v[:, k, m * 128:(m + 1) * 128], h2T[:, k, :], k == 0, k == 7, ["w13b%d" % b, "h2T"], ["ps%d" % m])
                ab = actT[b]
                for kc in range(2):
                    self.act(sil, self.bank(kc), AF.Silu, ["ps%d" % kc], ["sil"])
                    self.tt(ab[:, kc * 512:(kc + 1) * 512], sil, self.bank(2 + kc), ALU.mult, ["sil", "ps%d" % (2 + kc)], ["actT%d" % b])
                for j in range(4):
                    for cc in range(2):
                        bk = 4 + ycnt % 4
                        ycnt += 1
                        pk = "ps%d" % bk
                        for kc in range(2):
                            self.mm(self.bank(bk), ab[:, kc * 512 + j * 128:kc * 512 + (j + 1) * 128], w2v[:, kc, cc * 512:(cc + 1) * 512], kc == 0, kc == 1,
                                    ["actT%d" % b, "w2b%d" % b], [pk])
                        a_sl = acc[:, j, cc * 512:(cc + 1) * 512]
                        if e == 0:
                            self.ts(a_sl, self.bank(bk), Wr[:, j, e:e + 1], None, ALU.mult, None, [pk, "Wr"], ["acc%d" % j])
                        else:
                            self.stt(a_sl, self.bank(bk), Wr[:, j, e:e + 1], a_sl, ALU.mult, ALU.add, [pk, "Wr", "acc%d" % j], ["acc%d" % j])
            for j in range(4):
                b = j % 2
                c0 = g * 512 + j * 128
                self.tt(xo[b], acc[:, j, :], self.mod[:, 5120:6144], ALU.mult, ["acc%d" % j, "mod"], ["xo%d" % b])
                self.tt(xo[b], xo[b], xm[:, j, :], ALU.add, ["xo%d" % b, "xm%d" % j], ["xo%d" % b])
                d = self.dma(xout[c0:c0 + 128, :], xo[b], ["xo%d" % b], ["xout"], q="gpsimd")
                self.outs.append(d)


    def idma(self, out, in_, out_off, in_off, r, w):
        oo = bass.IndirectOffsetOnAxis(ap=out_off, axis=0) if out_off is not None else None
        io = bass.IndirectOffsetOnAxis(ap=in_off, axis=0) if in_off is not None else None
        return self.S.add("gpsimd", lambda e: e.indirect_dma_start(out=out, out_offset=oo, in_=in_, in_offset=io), r, w, dma=True)

    def ph_moe_sparse(self, l, xout):
        self.reset()
        Cd = self.Cd
        selAll = self.A(1024).rearrange("p (g e) -> p g e", g=32)
        WrAll = self.A(1024).rearrange("p (g e) -> p g e", g=32)
        CAll = self.A(1024).rearrange("p (g e) -> p g e", g=32)
        idxf = self.A(64)
        idxAll = self.A(64).bitcast(I32).rearrange("p (g k) -> p g k", g=32)
        wAll = self.A(64).rearrange("p (g k) -> p g k", g=32)
        tot = self.A(32)
        U = self.A(128)
        wr = self.A(8 * 36).rearrange("p (k n) -> p k n", k=8)
        brb = self.A(36)
        bstart = self.A(NBLK)
        off13 = self.A(8)
        off2 = self.A(2)
        pstart = self.A(32)
        pend = self.A(32)
        padf = self.A(32)
        cnti = self.A(32).bitcast(I32)
        blke = self.A(NBLK)
        w13if = self.A(NBLK * 8)
        w13i = self.A(NBLK * 8).bitcast(I32).rearrange("p (b k) -> p b k", k=8)
        w2if = self.A(NBLK * 2)
        w2i = self.A(NBLK * 2).bitcast(I32).rearrange("p (b k) -> p b k", k=2)
        h2T = self.A(1024).rearrange("p (k t) -> p k t", k=8)
        xm = [self.A(1024), self.A(1024)]
        ht = [self.A(1024), self.A(1024)]
        junk = self.A(1024)
        lg = self.A(36)
        stat = self.A(32)
        top8 = self.A(8)
        lfm = self.A(32)
        we = self.A(32)
        oh = self.A(4)
        gm = self.A(32)
        Dt = self.A(32)
        mh = self.A(32)
        self.dma(wr[:, :, 0:4], self.Wl("w_coarse", l).rearrange("(k p) n -> p k n", p=128), (), ["wr"])
        self.dma(wr[:, :, 4:36], self.Wl("w_fine", l).rearrange("(k p) n -> p k n", p=128), (), ["wr"])
        self.dma(brb[:, 0:4], self._bc_row(self.Wd["b_coarse"][l:l + 1, :], 4), (), ["brb"])
        self.dma(brb[:, 4:36], self._bc_row(self.Wd["b_fine"][l:l + 1, :], 32), (), ["brb"])
        self.dma(U, Cd["U"], (), ["U"])
        self.dma(bstart, Cd["bstart"], (), ["bstart"])
        self.dma(off13, Cd["off13"], (), ["off"])
        self.dma(off2, Cd["off2"], (), ["off"])
        self.mset(tot, 0.0, ["tot"])
        for gi in range(NT):
            b = gi % 2
            c0 = gi * 128
            self.dma(xm[b], self.xmid[c0:c0 + 128, :], ["xmid"], ["xm%d" % b])
            self.norm_tile(xm[b], ht[b], junk, stat, b, 4096, 3072, "xm%d" % b, "ht%d" % b)
            self.dma(self.H2d[c0:c0 + 128, :], ht[b], ["ht%d" % b], ["H2d"], q="gpsimd")
            self.transpose8(ht[b], h2T, 0, "ht%d" % b, "h2T", gi)
            for k in range(8):
                self.mm(self.bank(5)[:, 0:36], h2T[:, k, :], wr[:, k, :], k == 0, k == 7, ["h2T", "wr"], ["ps5"])
            self.tt(lg, self.bank(5)[:, 0:36], brb, ALU.add, ["ps5", "brb"], ["lg"])
            self.red(stat[:, 8:9], lg[:, 0:4], ALU.max, ["lg"], ["rs"])
            self.ts(oh, lg[:, 0:4], stat[:, 8:9], None, ALU.is_ge, None, ["lg", "rs"], ["oh"])
            self.ts(stat[:, 9:10], stat[:, 8:9], -1.0, None, ALU.mult, None, ["rs"], ["rs"])
            self.act(junk[:, 0:4], lg[:, 0:4], AF.Exp, ["lg", "rs"], ["junk", "rs"], bias=stat[:, 9:10], accum=stat[:, 10:11])
            self.ts(gm.rearrange("p (g e) -> p g e", g=4), oh.unsqueeze(2).to_broadcast([128, 4, 8]), 1e9, -1e9, ALU.mult, ALU.add, ["oh"], ["gm"])
            self.tt(lfm, lg[:, 4:36], gm, ALU.add, ["lg", "gm"], ["lfm"])
            self.vmax(top8, lfm, ["lfm"], ["top8"])
            sel = selAll[:, gi, :]
            self.ts(sel, lfm, top8[:, 1:2], None, ALU.is_ge, None, ["lfm", "top8"], ["sel"])
            self.ts(stat[:, 11:12], top8[:, 0:1], -1.0, None, ALU.mult, None, ["top8"], ["rs"])
            self.act(we, lfm, AF.Exp, ["lfm", "rs"], ["we"], bias=stat[:, 11:12])
            self.tt(we, we, sel, ALU.mult, ["we", "sel"], ["we"])
            self.red(stat[:, 12:13], we, ALU.add, ["we"], ["rs"])
            self.tt(stat[:, 13:14], stat[:, 12:13], stat[:, 10:11], ALU.mult, ["rs"], ["rs"])
            self.rcp(stat[:, 13:14], stat[:, 13:14], ["rs"], ["rs"])
            self.ts(WrAll[:, gi, :], we, stat[:, 13:14], None, ALU.mult, None, ["we", "rs"], ["Wr"])
            self.mm(self.bank(4)[:, 0:32], U, sel, True, True, ["U", "sel"], ["ps4"])
            self.mm(self.bank(4)[:, 32:64], self.ones, sel, True, True, ["ones", "sel"], ["ps4"])
            self.tt(CAll[:, gi, :], self.bank(4)[:, 0:32], tot, ALU.add, ["ps4", "tot"], ["CAll"])
            self.tt(tot, self.bank(4)[:, 32:64], tot, ALU.add, ["ps4", "tot"], ["tot"])
        self.ts(cnti, tot, 127.0, None, ALU.add, None, ["tot"], ["cnti"])
        self.S.add("vector", lambda e: e.tensor_scalar(out=cnti, in0=cnti, scalar1=7, scalar2=7, op0=ALU.arith_shift_right, op1=ALU.logical_shift_left), ["cnti"], ["cnti"])
        self.cp(padf, cnti, ["cnti"], ["padf"])
        self.S.add("vector", lambda e: e.tensor_tensor_scan(out=pend, data0=self.ones[:, 0:32], data1=padf, initial=0.0, op0=ALU.mult, op1=ALU.add), ["ones", "padf"], ["pend"])
        self.tt(pstart, pend, padf, ALU.subtract, ["pend", "padf"], ["pstart"])
        cmp3 = self.A(NBLK * 32).rearrange("p (b e) -> p b e", e=32)
        self.tt(cmp3, pend.unsqueeze(1).to_broadcast([128, NBLK, 32]), bstart.unsqueeze(2).to_broadcast([128, NBLK, 32]), ALU.is_le, ["pend", "bstart"], ["cmp3"])
        self.red(blke, cmp3, ALU.add, ["cmp3"], ["blke"])
        self.ts(blke, blke, 31.0, None, ALU.min, None, ["blke"], ["blke"])
        self.ts(w13if[:, 0:NBLK], blke, 128.0, float(l * 4096), ALU.mult, ALU.add, ["blke"], ["w13if"])
        self.tt(w13if[:, 0:NBLK], w13if[:, 0:NBLK], off13[:, 0:1].to_broadcast([128, NBLK]), ALU.add, ["w13if", "off"], ["w13if"])
        widx = w13i.rearrange("p b k -> p (b k)")[:, 0:NBLK]
        self.cp(widx, w13if[:, 0:NBLK], ["w13if"], ["widx"])
        hb = [self.A(1024), self.A(1024)]
        xskeys = []
        for gi in range(NT):
            b = gi % 2
            c0 = gi * 128
            self.tt(Dt, CAll[:, gi, :], pstart, ALU.add, ["CAll", "pstart"], ["Dt"])
            self.stt(Dt, Dt, 1.0, selAll[:, gi, :], ALU.add, ALU.mult, ["Dt", "sel"], ["Dt"])
            self.vmax(top8, Dt, ["Dt"], ["top8"])
            for k in range(2):
                self.ts(mh, Dt, top8[:, k:k + 1], None, ALU.is_equal, None, ["Dt", "top8"], ["mh"])
                self.tt(mh, mh, WrAll[:, gi, :], ALU.mult, ["mh", "Wr"], ["mh"])
                self.red(wAll[:, gi, k:k + 1], mh, ALU.add, ["mh"], ["wAll"])
            self.ts(idxf[:, 2 * gi:2 * gi + 2], top8[:, 0:2], -1.0, None, ALU.add, None, ["top8"], ["idxf"])
            self.cp(idxAll[:, gi, :], idxf[:, 2 * gi:2 * gi + 2], ["idxf"], ["idxAll"])
            self.dma(hb[b], self.H2d[c0:c0 + 128, :], ["H2d"], ["hb%d" % b])
            for k in range(2):
                key = "Xs%d_%d" % (gi, k)
                xskeys.append(key)
                self.idma(self.Xsort[:, :], hb[b], idxAll[:, gi, k:k + 1], None, ["hb%d" % b, "idxAll"], [key])
        xb = [self.A(1024), self.A(1024)]
        xbT = self.A(1024).rearrange("p (k t) -> p k t", k=8)
        w13b = [self.A(4096).rearrange("p (k n) -> p k n", k=8) for _ in range(2)]
        w2b = [self.A(2048).rearrange("p (k d) -> p k d", k=2) for _ in range(2)]
        sil = self.A(256)
        actv = self.A(256)
        actT = self.A(256).rearrange("p (k t) -> p k t", k=2)
        ysb = [self.A(1024), self.A(1024)]
        w13flat = self.Wd["w13"].rearrange("l e (p k) n -> (l e p) (k n)", k=8)
        w2flat = self.Wd["w2"].rearrange("l e (p k) d -> (l e p) (k d)", k=2)
        yskeys = []
        cnt = 0
        for bi in range(NBLK):
            b = bi % 2
            self.dma(xb[b], self.Xsort[bi * 128:(bi + 1) * 128, :], xskeys, ["xb%d" % b])
            self.idma(w13b[b].rearrange("p k n -> p (k n)"), w13flat[:, :], None, widx[:, bi:bi + 1], ["widx"], ["w13b%d" % b])
            self.idma(w2b[b].rearrange("p k d -> p (k d)"), w2flat[:, :], None, widx[:, bi:bi + 1], ["widx"], ["w2b%d" % b])
            for j in range(2):
                tbk = 6 + j
                for kk in range(4):
                    k = 4 * j + kk
                    self.tr(self.bank(tbk)[:, kk * 128:(kk + 1) * 128], xb[b][:, k:1024:8], ["xb%d" % b, "ident"], ["ps%d" % tbk])
                self.cp(xbT[:, 4 * j:4 * j + 4, :], self.bank(tbk).rearrange("p (k t) -> p k t", k=4), ["ps%d" % tbk], ["xbT"],
                        eng=("scalar" if (j + bi) % 2 == 0 else "vector"))
            bk = cnt % 4
            cnt += 1
            for k in range(8):
                self.mm(self.bank(bk), xbT[:, k, :], w13b[b][:, k, :], k == 0, k == 7, ["xbT", "w13b%d" % b], ["ps%d" % bk])
            self.act(sil, self.bank(bk)[:, 0:256], AF.Silu, ["ps%d" % bk], ["sil"])
            self.tt(actv, sil, self.bank(bk)[:, 256:512], ALU.mult, ["sil", "ps%d" % bk], ["actv"])
            for kc in range(2):
                self.tr(self.bank(5)[:, kc * 128:(kc + 1) * 128], actv[:, kc:256:2], ["actv", "ident"], ["ps5"])
            self.cp(actT.rearrange("p k t -> p (k t)"), self.bank(5)[:, 0:256], ["ps5"], ["actT"], eng="scalar")
            for cc in range(2):
                bk = cnt % 4
                cnt += 1
                for kc in range(2):
                    self.mm(self.bank(bk), actT[:, kc, :], w2b[b][:, kc, cc * 512:(cc + 1) * 512], kc == 0, kc == 1, ["actT", "w2b%d" % b], ["ps%d" % bk])
                self.cp(ysb[b][:, cc * 512:(cc + 1) * 512], self.bank(bk), ["ps%d" % bk], ["ysb%d" % b], eng=("scalar" if cc == 0 else "vector"))
            key = "Ys%d" % bi
            yskeys.append(key)
            self.dma(self.Ysort[bi * 128:(bi + 1) * 128, :], ysb[b], ["ysb%d" % b], [key], q="sync")
        Y = [[self.A(1024), self.A(1024)] for _ in range(2)]
        xo = [self.A(1024), self.A(1024)]
        for gi in range(NT):
            b = gi % 2
            c0 = gi * 128
            self.dma(xm[b], self.xmid[c0:c0 + 128, :], ["xmid"], ["xm%d" % b])
            for k in range(2):
                self.idma(Y[b][k], self.Ysort[:, :], None, idxAll[:, gi, k:k + 1], yskeys + ["idxAll"], ["Y%d%d" % (b, k)])
            self.ts(xo[b], Y[b][0], wAll[:, gi, 0:1], None, ALU.mult, None, ["Y%d0" % b, "wAll"], ["xo%d" % b])
            self.stt(xo[b], Y[b][1], wAll[:, gi, 1:2], xo[b], ALU.mult, ALU.add, ["Y%d1" % b, "wAll", "xo%d" % b], ["xo%d" % b])
            self.tt(xo[b], xo[b], self.mod[:, 5120:6144], ALU.mult, ["xo%d" % b, "mod"], ["xo%d" % b])
            self.tt(xo[b], xo[b], xm[b], ALU.add, ["xo%d" % b, "xm%d" % b], ["xo%d" % b])
            d = self.dma(xout[c0:c0 + 128, :], xo[b], ["xo%d" % b], ["xout"], q="sync")
            self.outs.append(d)

_CONSTS = None


def _consts():
    global _CONSTS
    if _CONSTS is None:
        _CONSTS = make_consts()
    return _CONSTS


def _in_map(x_b, c_b, weights, layers):
    m = {"x": np.ascontiguousarray(x_b, dtype=np.float32), "c": np.ascontiguousarray(c_b.reshape(8, 128), dtype=np.float32)}
    for name, _ in WEIGHT_SPECS:
        m[name] = np.ascontiguousarray(weights[name][layers])
    for name, _, _ in CONST_SPECS:
        m["k_" + name] = _consts()[name]
    return m


FUSED = True


def kernel(**inputs):
    x = np.asarray(inputs["x"], dtype=np.float32)
    c = np.asarray(inputs["c"], dtype=np.float32)
    weights = {name: np.asarray(inputs[name], dtype=np.float32) for name, _ in WEIGHT_SPECS}
    n = 8
    if FUSED:
        nc = Builder(DEPTH).build()
        layers = list(range(DEPTH))
        in_maps = [_in_map(x[i % 4], c[i % 4], weights, layers) for i in range(n)]
        res = run_bass_kernel_spmd(nc, in_maps, core_ids=list(range(n)))
        return np.stack([res.results[i]["y"] for i in range(4)], axis=0).astype(np.float32)
    nc = Builder(1).build()
    cur = [x[i] for i in range(4)]
    for l in range(DEPTH):
        in_maps = [_in_map(cur[i % 4], c[i % 4], weights, [l]) for i in range(n)]
        res = run_bass_kernel_spmd(nc, in_maps, core_ids=list(range(n)))
        cur = [np.asarray(res.results[i]["y"]) for i in range(4)]
    return np.stack(cur, axis=0).astype(np.float32)
```
